# Optimizing a Trainium2 kernel written in Bass

```python
import numpy as np
import jax
import jax.numpy as jnp
from jax import lax

D_MODEL = 2048
BATCH = 8
SEQ = 4096
DEPTH = 2

RMS_EPS = 1e-6
MIX_WIDTH = D_MODEL

S5_WIDTH = D_MODEL // 4
S5_GROUP_CH = 16
S5_GROUPS = S5_WIDTH // S5_GROUP_CH
S5_STATE = 64

SSD_WIDTH = MIX_WIDTH - S5_WIDTH
SSD_HEADDIM = 64
SSD_HEADS = SSD_WIDTH // SSD_HEADDIM
SSD_GROUPS = 4
SSD_HPG = SSD_HEADS // SSD_GROUPS
SSD_STATE = 128
SSD_CONV = 4
SSD_CHUNK = 128
SSD_CONV_CH = SSD_WIDTH + 2 * SSD_GROUPS * SSD_STATE

SWA_HEADDIM = 64
SWA_WIDTH = MIX_WIDTH // 2
SWA_HEADS = SWA_WIDTH // SWA_HEADDIM
SWA_KV_HEADS = 4
SWA_HPG = SWA_HEADS // SWA_KV_HEADS
SWA_WINDOW = 128

GLA_VALUE_WIDTH = MIX_WIDTH - SWA_WIDTH
GLA_KEY_WIDTH = GLA_VALUE_WIDTH // 2
GLA_HEADS = 4
GLA_HEAD_K = GLA_KEY_WIDTH // GLA_HEADS
GLA_HEAD_V = GLA_VALUE_WIDTH // GLA_HEADS
GLA_GATE_RANK = 16
GLA_GATE_NORMALIZER = 16.0
GLA_CHUNK = 64

FFN_HIDDEN = -(-(8 * D_MODEL) // (3 * 256)) * 256

IN0_SIZES = (S5_WIDTH, SSD_WIDTH, SSD_CONV_CH, SSD_HEADS)
IN1_SIZES = (SWA_HEADS * SWA_HEADDIM, SWA_KV_HEADS * SWA_HEADDIM, SWA_KV_HEADS * SWA_HEADDIM,
             GLA_KEY_WIDTH, GLA_KEY_WIDTH, GLA_VALUE_WIDTH, GLA_VALUE_WIDTH, GLA_GATE_RANK)

kernel_name = "hybrid_s5_ssd_swa_gla_block"


def split_cols(t, sizes):
    return jnp.split(t, np.cumsum(sizes)[:-1].tolist(), axis=-1)


def rmsnorm(t, w):
    tf = t.astype(jnp.float32)
    y = tf * lax.rsqrt(jnp.mean(tf * tf, axis=-1, keepdims=True) + RMS_EPS)
    return (y * w.astype(jnp.float32)).astype(t.dtype)


def swiglu(t, w1, w3, w2):
    return (jax.nn.silu(t @ w1) * (t @ w3)) @ w2


def causal_depthwise_conv(t, w, bias):
    k_w, ch = w.shape
    out = lax.conv_general_dilated(t, w[:, None, :].astype(t.dtype), window_strides=(1,),
                                   padding=[(k_w - 1, 0)],
                                   dimension_numbers=('NWC', 'WIO', 'NWC'),
                                   feature_group_count=ch)
    return out + bias


def s5_mixer(u, A_re, A_im, log_dt, B_re, B_im, C_re, C_im, D, glu_w, glu_b):
    f32 = jnp.float32
    b, L, _ = u.shape
    uf = u.astype(f32)
    ug = uf.reshape(b, L, S5_GROUPS, S5_GROUP_CH)
    a_re, a_im = A_re.astype(f32), A_im.astype(f32)
    dt = jnp.exp(log_dt.astype(f32))[:, None]
    mag = jnp.exp(a_re * dt)
    ab_re = mag * jnp.cos(a_im * dt)
    ab_im = mag * jnp.sin(a_im * dt)
    e_re, e_im = ab_re - 1.0, ab_im
    den = a_re * a_re + a_im * a_im
    coef_re = (e_re * a_re + e_im * a_im) / den
    coef_im = (e_im * a_re - e_re * a_im) / den
    bu_re = jnp.einsum('blgc,gnc->blgn', ug, B_re.astype(f32))
    bu_im = jnp.einsum('blgc,gnc->blgn', ug, B_im.astype(f32))
    x_re = coef_re * bu_re - coef_im * bu_im
    x_im = coef_re * bu_im + coef_im * bu_re
    a_re_seq = jnp.broadcast_to(ab_re, (1, L) + ab_re.shape)
    a_im_seq = jnp.broadcast_to(ab_im, (1, L) + ab_im.shape)

    def combine(e1, e2):
        a1r, a1i, b1r, b1i = e1
        a2r, a2i, b2r, b2i = e2
        return (a2r * a1r - a2i * a1i,
                a2r * a1i + a2i * a1r,
                a2r * b1r - a2i * b1i + b2r,
                a2r * b1i + a2i * b1r + b2i)

    _, _, h_re, h_im = lax.associative_scan(combine, (a_re_seq, a_im_seq, x_re, x_im), axis=1)
    y = (jnp.einsum('blgn,gcn->blgc', h_re, C_re.astype(f32))
         - jnp.einsum('blgn,gcn->blgc', h_im, C_im.astype(f32)))
    y = y.reshape(b, L, S5_WIDTH) + D.astype(f32) * uf
    y = jax.nn.gelu(y)
    y = y * jax.nn.sigmoid(y @ glu_w.astype(f32) + glu_b.astype(f32))
    return y.astype(u.dtype)


def ssd_mixer(z, xbc, dt_raw, conv_w, conv_b, dt_bias, A_log, D, norm_w):
    f32 = jnp.float32
    b, L, _ = z.shape
    nc, Q = L // SSD_CHUNK, SSD_CHUNK
    G, R, P, N = SSD_GROUPS, SSD_HPG, SSD_HEADDIM, SSD_STATE
    xbc = jax.nn.silu(causal_depthwise_conv(xbc, conv_w, conv_b)).astype(f32)
    xs, Bm, Cm = split_cols(xbc, (SSD_WIDTH, G * N, G * N))
    xs = xs.reshape(b, nc, Q, G, R, P)
    Bm = Bm.reshape(b, nc, Q, G, N)
    Cm = Cm.reshape(b, nc, Q, G, N)
    dt = jax.nn.softplus(dt_raw.astype(f32) + dt_bias.astype(f32)).reshape(b, nc, Q, G, R)
    A = -jnp.exp(A_log.astype(f32)).reshape(G, R)
    cs = jnp.moveaxis(jnp.cumsum(dt * A, axis=2), 2, -1)
    xdt = xs * dt[..., None]
    pos = jnp.arange(Q)
    causal = pos[:, None] >= pos[None, :]
    seg = jnp.where(causal, cs[..., :, None] - cs[..., None, :], -jnp.inf)
    cb = jnp.einsum('bclgn,bcsgn->bcgls', Cm, Bm)
    y_diag = jnp.einsum('bcgls,bcgrls,bcsgrp->bclgrp', cb, jnp.exp(seg), xdt)
    cs_last = cs[..., -1:]
    states = jnp.einsum('bclgn,bcgrl,bclgrp->bcgrpn', Bm, jnp.exp(cs_last - cs), xdt)
    chunk_decay = jnp.exp(cs_last[..., 0])

    def step(S, inp):
        dec, st = inp
        return dec[..., None, None] * S + st, S

    S0 = jnp.zeros((b, G, R, P, N), f32)
    _, S_in = lax.scan(step, S0, (jnp.moveaxis(chunk_decay, 1, 0), jnp.moveaxis(states, 1, 0)))
    S_in = jnp.moveaxis(S_in, 0, 1)
    y_off = jnp.einsum('bclgn,bcgrpn,bcgrl->bclgrp', Cm, S_in, jnp.exp(cs))
    y = y_diag + y_off + D.astype(f32).reshape(G, R)[..., None] * xs
    y = y.reshape(b, L, SSD_WIDTH) * jax.nn.silu(z.astype(f32))
    y = rmsnorm(y.reshape(b, L, G, SSD_WIDTH // G), norm_w.reshape(G, SSD_WIDTH // G))
    return y.reshape(b, L, SSD_WIDTH).astype(z.dtype)


def swa_sink_attention(q, k, v, q_norm_w, k_norm_w, sinks):
    f32 = jnp.float32
    b, L, _ = q.shape
    blk = SWA_WINDOW
    nb = L // blk
    H, KV, R, HD = SWA_HEADS, SWA_KV_HEADS, SWA_HPG, SWA_HEADDIM
    q = rmsnorm(q.reshape(b, L, H, HD), q_norm_w).reshape(b, nb, blk, KV, R, HD)
    k = rmsnorm(k.reshape(b, L, KV, HD), k_norm_w).reshape(b, nb, blk, KV, HD)
    v = v.reshape(b, nb, blk, KV, HD)

    def with_prev(t):
        prev = jnp.concatenate([jnp.zeros_like(t[:, :1]), t[:, :-1]], axis=1)
        return jnp.concatenate([prev, t], axis=2)

    kw, vw = with_prev(k), with_prev(v)
    s = jnp.einsum('bnqhrd,bnkhd->bnhrqk', q, kw).astype(f32) * (HD ** -0.5)
    qpos = jnp.arange(blk)[:, None]
    kpos = jnp.arange(2 * blk)[None, :] - blk
    rel = qpos - kpos
    blk_start = (jnp.arange(nb) * blk)[:, None, None]
    valid = (rel >= 0) & (rel < SWA_WINDOW) & (blk_start + kpos >= 0)
    s = jnp.where(valid[None, :, None, None], s, -jnp.inf)
    sink = sinks.astype(f32).reshape(KV, R)[None, None, :, :, None, None]
    m = jnp.maximum(jnp.max(s, axis=-1, keepdims=True), sink)
    p = jnp.exp(s - m)
    denom = jnp.sum(p, axis=-1, keepdims=True) + jnp.exp(sink - m)
    o = jnp.einsum('bnhrqk,bnkhd->bnqhrd', (p / denom).astype(vw.dtype), vw)
    return o.reshape(b, L, SWA_WIDTH)


def gla_mixer(q, k, v, gout, g_lr, gate_w2, gate_b, norm_w):
    f32 = jnp.float32
    b, L, _ = q.shape
    C = GLA_CHUNK
    nc = L // C
    H, DK, DV = GLA_HEADS, GLA_HEAD_K, GLA_HEAD_V
    q = q.astype(f32).reshape(b, nc, C, H, DK) * (DK ** -0.5)
    k = k.astype(f32).reshape(b, nc, C, H, DK)
    v = v.astype(f32).reshape(b, nc, C, H, DV)
    gk = jax.nn.log_sigmoid((g_lr @ gate_w2 + gate_b).astype(f32)) / GLA_GATE_NORMALIZER
    bc = jnp.cumsum(gk.reshape(b, nc, C, H, DK), axis=2)
    q_t = q * jnp.exp(bc)
    k_t = k * jnp.exp(-bc)
    pos = jnp.arange(C)
    causal = pos[:, None] >= pos[None, :]
    att = jnp.where(causal, jnp.einsum('bclhd,bcshd->bchls', q_t, k_t), 0.0)
    o_intra = jnp.einsum('bchls,bcshv->bclhv', att, v)
    b_last = bc[:, :, -1:]
    chunk_kv = jnp.einsum('bclhd,bclhv->bchdv', k * jnp.exp(b_last - bc), v)
    chunk_decay = jnp.exp(b_last[:, :, 0])

    def step(S, inp):
        dec, kv = inp
        return dec[..., None] * S + kv, S

    S0 = jnp.zeros((b, H, DK, DV), f32)
    _, S_in = lax.scan(step, S0, (jnp.moveaxis(chunk_decay, 1, 0), jnp.moveaxis(chunk_kv, 1, 0)))
    S_in = jnp.moveaxis(S_in, 0, 1)
    o = o_intra + jnp.einsum('bclhd,bchdv->bclhv', q_t, S_in)
    o = rmsnorm(o, norm_w).reshape(b, L, GLA_VALUE_WIDTH)
    return (o * jax.nn.silu(gout.astype(f32))).astype(gout.dtype)


def setup_inputs(seed: int = 0) -> dict:
    key = jax.random.key(seed)
    ks = iter(jax.random.split(key, 48))
    f32 = jnp.float32

    def nrm(shape, scale):
        return jax.random.normal(next(ks), shape, f32) * scale

    def gain(shape):
        return 1.0 + nrm(shape, 0.01)

    def log_uniform(shape, lo, hi):
        u = jax.random.uniform(next(ks), shape, f32)
        return u * (np.log(hi) - np.log(lo)) + np.log(lo)

    x = nrm((BATCH, SEQ, D_MODEL), 1.0)
    norm0_mix = gain((D_MODEL,))
    w_in0 = nrm((D_MODEL, sum(IN0_SIZES)), D_MODEL ** -0.5)
    s5_A_re = -0.5 + nrm((S5_GROUPS, S5_STATE), 0.01)
    s5_A_im = jnp.pi * jnp.arange(S5_STATE, dtype=f32)[None, :] + nrm((S5_GROUPS, S5_STATE), 0.01)
    s5_log_dt = log_uniform((S5_GROUPS,), 1e-3, 1e-1)
    s5_B_re = nrm((S5_GROUPS, S5_STATE, S5_GROUP_CH), (2 * S5_GROUP_CH) ** -0.5)
    s5_B_im = nrm((S5_GROUPS, S5_STATE, S5_GROUP_CH), (2 * S5_GROUP_CH) ** -0.5)
    s5_C_re = nrm((S5_GROUPS, S5_GROUP_CH, S5_STATE), S5_STATE ** -0.5)
    s5_C_im = nrm((S5_GROUPS, S5_GROUP_CH, S5_STATE), S5_STATE ** -0.5)
    s5_D = nrm((S5_WIDTH,), 1.0)
    s5_glu_w = nrm((S5_WIDTH, S5_WIDTH), S5_WIDTH ** -0.5)
    s5_glu_b = nrm((S5_WIDTH,), 0.01)
    ssd_conv_w = nrm((SSD_CONV, SSD_CONV_CH), SSD_CONV ** -0.5)
    ssd_conv_b = nrm((SSD_CONV_CH,), 0.01)
    dt0 = jnp.exp(log_uniform((SSD_HEADS,), 1e-3, 1e-1))
    ssd_dt_bias = dt0 + jnp.log(-jnp.expm1(-dt0))
    ssd_A_log = jnp.log(jax.random.uniform(next(ks), (SSD_HEADS,), f32, 1.0, 16.0))
    ssd_D = gain((SSD_HEADS,))
    ssd_norm_w = gain((SSD_WIDTH,))
    w_out0 = nrm((MIX_WIDTH, D_MODEL), MIX_WIDTH ** -0.5)
    norm0_ffn = gain((D_MODEL,))
    ffn0_w1 = nrm((D_MODEL, FFN_HIDDEN), D_MODEL ** -0.5)
    ffn0_w3 = nrm((D_MODEL, FFN_HIDDEN), D_MODEL ** -0.5)
    ffn0_w2 = nrm((FFN_HIDDEN, D_MODEL), FFN_HIDDEN ** -0.5)
    norm1_mix = gain((D_MODEL,))
    w_in1 = nrm((D_MODEL, sum(IN1_SIZES)), D_MODEL ** -0.5)
    swa_q_norm = gain((SWA_HEADDIM,))
    swa_k_norm = gain((SWA_HEADDIM,))
    swa_sinks = nrm((SWA_HEADS,), 0.5)
    gla_gate_w2 = nrm((GLA_GATE_RANK, GLA_KEY_WIDTH), GLA_GATE_RANK ** -0.5)
    gla_gate_b = nrm((GLA_KEY_WIDTH,), 0.01)
    gla_norm_w = gain((GLA_HEAD_V,))
    w_out1 = nrm((MIX_WIDTH, D_MODEL), MIX_WIDTH ** -0.5)
    norm1_ffn = gain((D_MODEL,))
    ffn1_w1 = nrm((D_MODEL, FFN_HIDDEN), D_MODEL ** -0.5)
    ffn1_w3 = nrm((D_MODEL, FFN_HIDDEN), D_MODEL ** -0.5)
    ffn1_w2 = nrm((FFN_HIDDEN, D_MODEL), FFN_HIDDEN ** -0.5)
    return {
        'x': x,
        'norm0_mix': norm0_mix, 'w_in0': w_in0,
        's5_A_re': s5_A_re, 's5_A_im': s5_A_im, 's5_log_dt': s5_log_dt,
        's5_B_re': s5_B_re, 's5_B_im': s5_B_im, 's5_C_re': s5_C_re, 's5_C_im': s5_C_im,
        's5_D': s5_D, 's5_glu_w': s5_glu_w, 's5_glu_b': s5_glu_b,
        'ssd_conv_w': ssd_conv_w, 'ssd_conv_b': ssd_conv_b, 'ssd_dt_bias': ssd_dt_bias,
        'ssd_A_log': ssd_A_log, 'ssd_D': ssd_D, 'ssd_norm_w': ssd_norm_w,
        'w_out0': w_out0, 'norm0_ffn': norm0_ffn,
        'ffn0_w1': ffn0_w1, 'ffn0_w3': ffn0_w3, 'ffn0_w2': ffn0_w2,
        'norm1_mix': norm1_mix, 'w_in1': w_in1,
        'swa_q_norm': swa_q_norm, 'swa_k_norm': swa_k_norm, 'swa_sinks': swa_sinks,
        'gla_gate_w2': gla_gate_w2, 'gla_gate_b': gla_gate_b, 'gla_norm_w': gla_norm_w,
        'w_out1': w_out1, 'norm1_ffn': norm1_ffn,
        'ffn1_w1': ffn1_w1, 'ffn1_w3': ffn1_w3, 'ffn1_w2': ffn1_w2,
    }


def reference(x, norm0_mix, w_in0, s5_A_re, s5_A_im, s5_log_dt, s5_B_re, s5_B_im, s5_C_re,
              s5_C_im, s5_D, s5_glu_w, s5_glu_b, ssd_conv_w, ssd_conv_b, ssd_dt_bias, ssd_A_log,
              ssd_D, ssd_norm_w, w_out0, norm0_ffn, ffn0_w1, ffn0_w3, ffn0_w2, norm1_mix, w_in1,
              swa_q_norm, swa_k_norm, swa_sinks, gla_gate_w2, gla_gate_b, gla_norm_w, w_out1,
              norm1_ffn, ffn1_w1, ffn1_w3, ffn1_w2):
    h = x
    for layer in range(DEPTH):
        if layer % 2 == 0:
            proj = rmsnorm(h, norm0_mix) @ w_in0
            u, z, xbc, dt_raw = split_cols(proj, IN0_SIZES)
            ya = s5_mixer(u, s5_A_re, s5_A_im, s5_log_dt, s5_B_re, s5_B_im, s5_C_re, s5_C_im,
                          s5_D, s5_glu_w, s5_glu_b)
            yb = ssd_mixer(z, xbc, dt_raw, ssd_conv_w, ssd_conv_b, ssd_dt_bias, ssd_A_log,
                           ssd_D, ssd_norm_w)
            h = h + (jnp.concatenate([ya, yb], axis=-1) @ w_out0).astype(h.dtype)
            h = h + swiglu(rmsnorm(h, norm0_ffn), ffn0_w1, ffn0_w3, ffn0_w2).astype(h.dtype)
        else:
            proj = rmsnorm(h, norm1_mix) @ w_in1
            qc, kc, vc, qd, kd, vd, gout, g_lr = split_cols(proj, IN1_SIZES)
            yc = swa_sink_attention(qc, kc, vc, swa_q_norm, swa_k_norm, swa_sinks)
            yd = gla_mixer(qd, kd, vd, gout, g_lr, gla_gate_w2, gla_gate_b, gla_norm_w)
            h = h + (jnp.concatenate([yc.astype(h.dtype), yd.astype(h.dtype)], axis=-1) @ w_out1).astype(h.dtype)
            h = h + swiglu(rmsnorm(h, norm1_ffn), ffn1_w1, ffn1_w3, ffn1_w2).astype(h.dtype)
    return h
```

```python
import numpy as np
import concourse.bass as bass
import concourse.mybir as mybir
from concourse.bass_utils import run_bass_kernel_spmd
from contextlib import ExitStack

F32 = mybir.dt.float32
BF16 = mybir.dt.bfloat16
I32 = mybir.dt.int32
AF = mybir.ActivationFunctionType
ALU = mybir.AluOpType

D = 2048
KT = 16
TT = 512
FH = 5632
HT = 44
NB = TT // 128
EPS = 1e-6
PI = float(np.pi)
NDMA = 8
WTOT = (2048 * 4632 + 2048 * 4624 + 2 * 2048 * 2048 + 6 * 2048 * 5632) // 128


class Op:
    __slots__ = ("eng", "fn", "deps", "isdma", "flag", "sem", "val")


class Sched:
    ENGS = ("pe", "act", "dve", "pool", "sp")

    def __init__(self):
        self.ops = {e: [] for e in self.ENGS}
        self.last_w = {}
        self.readers = {}
        self.dmas = {e: [] for e in self.ENGS}

    def add(self, eng, fn, reads=(), writes=(), dma=False):
        op = Op()
        op.eng, op.fn, op.isdma, op.flag, op.sem, op.val = eng, fn, dma, False, None, 0
        deps = {}
        for r in reads:
            w = self.last_w.get(r)
            if w is not None:
                deps[id(w)] = (w, True)
        for k in writes:
            w = self.last_w.get(k)
            if w is not None and id(w) not in deps:
                deps[id(w)] = (w, False)
            for rd in self.readers.get(k, ()):
                if id(rd) not in deps:
                    deps[id(rd)] = (rd, False)
        final = []
        for d, raw in deps.values():
            if d.eng == eng and not d.isdma and not dma:
                if (not raw) or eng == "pe":
                    continue
            final.append(d)
        if dma:
            lst = self.dmas[eng]
            if len(lst) >= NDMA:
                final.append(lst[-NDMA])
            lst.append(op)
        op.deps = final
        for r in reads:
            self.readers.setdefault(r, []).append(op)
        for k in writes:
            self.last_w[k] = op
            self.readers[k] = []
        self.ops[eng].append(op)
        return op

    PERSIST = ("wbuf", "wscr", "h", "out")
    BENGS = ("pe", "act", "dve", "pool")

    def barrier(self, full=False):
        engs = self.ENGS if full else self.BENGS
        lasts = []
        for e in engs:
            real = [o for o in self.ops[e][-3:] if o.fn is not None and not o.isdma]
            if real:
                lasts.append(real[-1])
            if full:
                lasts.extend(self.dmas[e][-NDMA:])
        for e in engs:
            op = Op()
            op.eng, op.fn, op.isdma, op.flag, op.sem, op.val = e, None, False, False, None, 0
            op.deps = [d for d in lasts if not (d.eng == e and not d.isdma)]
            self.ops[e].append(op)
        keep = lambda k: isinstance(k, tuple) and k[0] in self.PERSIST
        self.last_w = {k: v for k, v in self.last_w.items() if keep(k)}
        self.readers = {k: v for k, v in self.readers.items() if keep(k)}

    def emit(self, nc, es):
        csem = {e: es.enter_context(nc.semaphore("c_" + e)) for e in self.ENGS}
        dsem = {e: [es.enter_context(nc.semaphore("d_%s%d" % (e, i))) for i in range(NDMA)] for e in ("sp", "pool", "act")}
        for e in self.ENGS:
            for op in self.ops[e]:
                for d in op.deps:
                    d.flag = True
        for e in self.ENGS:
            cnt = 0
            k = 0
            for op in self.ops[e]:
                if op.isdma:
                    op.sem = dsem[e][k % NDMA]
                    op.val = 16 * (k // NDMA + 1)
                    k += 1
                elif op.flag:
                    if op.fn is None:
                        raise RuntimeError("dep on barrier op")
                    cnt += 1
                    op.sem = csem[e]
                    op.val = cnt
        block = es.enter_context(nc.Block())

        def run(e, eng):
            seen = {}
            for op in self.ops[e]:
                for d in op.deps:
                    if seen.get(d.sem, 0) < d.val:
                        eng.wait_ge(d.sem, d.val)
                        seen[d.sem] = d.val
                if op.fn is None:
                    continue
                ins = op.fn(eng)
                if op.isdma:
                    ins.then_inc(op.sem, 16)
                elif op.flag:
                    ins.then_inc(op.sem, 1)

        @block.tensor
        def _(eng):
            run("pe", eng)

        @block.scalar
        def _(eng):
            run("act", eng)

        @block.vector
        def _(eng):
            run("dve", eng)

        @block.gpsimd
        def _(eng):
            run("pool", eng)

        @block.sync
        def _(eng):
            run("sp", eng)


def _keys(name, idxs):
    return [(name, i) for i in idxs]


class Builder:
    def __init__(self, T, layers=(0, 1), dbg=None):
        self.T = T
        self.NT = T // TT
        self.layers = layers
        self.dbg = dbg
        self.nc = bass.Bass("TRN2", target_bir_lowering=False)
        self.S = Sched()
        self.din = {}
        self.wrr = 0
        self.wmap = {}
        self.wreg = []
        self.wtot = 0

    def sb(self, name, shape, dt):
        self.uid = getattr(self, "uid", 0) + 1
        return self.nc.sbuf_tensor("%s_%d" % (name, self.uid), shape, dt)

    def dram_in(self, name, shape):
        ap = self.nc.dram_tensor(name, list(shape), F32, kind="ExternalInput").ap()
        self.din[name] = ap
        return ap

    def mm(self, out, lhsT, rhs, start, stop, r, w):
        self.S.add("pe", lambda e: e.matmul(out, lhsT=lhsT, rhs=rhs, start=start, stop=stop), r, w)

    def tr(self, out, in_, ident, r, w):
        self.S.add("pe", lambda e: e.transpose(out, in_, ident), r, w)

    def act(self, out, in_, func, r, w, **kw):
        self.S.add("act", lambda e: e.activation(out=out, in_=in_, func=func, **kw), r, w)

    def tt(self, eng, out, in0, in1, op, r, w):
        self.S.add(eng, lambda e: e.tensor_tensor(out=out, in0=in0, in1=in1, op=op), r, w)

    def ts(self, eng, out, in0, s1, s2, op0, op1, r, w):
        if op1 is None:
            self.S.add(eng, lambda e: e.tensor_scalar(out=out, in0=in0, scalar1=s1, scalar2=None, op0=op0), r, w)
        else:
            self.S.add(eng, lambda e: e.tensor_scalar(out=out, in0=in0, scalar1=s1, scalar2=s2, op0=op0, op1=op1), r, w)

    def stt(self, eng, out, in0, scalar, in1, op0, op1, r, w):
        self.S.add(eng, lambda e: e.scalar_tensor_tensor(out=out, in0=in0, scalar=scalar, in1=in1, op0=op0, op1=op1), r, w)

    def cp(self, eng, out, in_, r, w):
        if eng == "act":
            self.S.add("act", lambda e: e.activation(out=out, in_=in_, func=AF.Copy), r, w)
        else:
            self.S.add(eng, lambda e: e.tensor_copy(out=out, in_=in_), r, w)

    def memset(self, eng, ap, val, w):
        self.S.add(eng, lambda e: e.memset(ap, val), (), w)

    def dma(self, eng, out, in_, r, w):
        self.S.add(eng, lambda e: e.dma_start(out=out, in_=in_), r, w, dma=True)

    def load(self, out, in_, w, cast=False):
        self.dma("pool" if cast else "sp", out, in_, (), w)

    def stream_w(self, wname, c0, ncols, nk=KT, k0=0, pieces=None):
        if pieces is None:
            pieces = ((wname, c0, ncols),)
        key = (tuple(pieces), k0, nk)
        ncols = sum(p[2] for p in pieces)
        n = nk * ncols
        if key not in self.wmap:
            self.wmap[key] = (len(self.wreg), self.wtot)
            self.wreg.append((tuple(pieces), k0, nk, self.wtot, n))
            off = self.wtot
            self.wtot += n
            bid = self.wmap[key][0]
            self.dma("pool", self.wscr[:, off:off + n], self.wpack[:, off:off + n], (), [("wscr", bid)])
        bid, off = self.wmap[key]
        i = self.wrr % len(self.wbuf)
        self.wrr += 1
        buf = self.wbuf[i]
        view = buf[:, 0:n].rearrange("p (k n) -> p k n", n=ncols)
        self.dma("sp", buf[:, 0:n], self.wscr[:, off:off + n], [("wscr", bid)], [("wbuf", i)])
        return view, ("wbuf", i)

    def rmsnorm(self, wcol, xn):
        h, sq, ps, rstd = self.h, self.sq, self.ps, self.rstd
        for k in range(KT):
            self.act(sq[:, k % 2, :], h[:, k, :], AF.Square, [("h", k)], [("sq", k % 2)])
            self.mm(ps[6][:, 0:TT], self.ones_bf[:], sq[:, k % 2, :], k == 0, k == KT - 1, [("sq", k % 2), "consts"], [("ps", 6)])
        self.act(rstd[:], ps[6][:, 0:TT], AF.Ln, [("ps", 6)], ["rstd"], scale=1.0 / D, bias=self.eps_col[:, 0:1])
        self.act(rstd[:], rstd[:], AF.Exp, ["rstd"], ["rstd"], scale=-0.5)
        for k in range(KT):
            self.stt("dve", xn[:, k, :], h[:, k, :], wcol[:, k:k + 1], rstd[:], ALU.mult, ALU.mult,
                     [("h", k), "rstd", "params"], [("xn", k)])

    def proj_fm(self, Wd, c0, ntiles, xn, evac, nk=KT, xkeys=None):
        if xkeys is None:
            xkeys = [("xn", k) for k in range(nk)]
        if isinstance(xn, list):
            class _L:
                def __init__(s_, l):
                    s_.l = l
                def __getitem__(s_, idx):
                    return s_.l[idx[1]]
            xn = _L(xn)
        j = 0
        if nk > KT:
            nh = nk // 2
            for j in range(ntiles):
                b = self.gb % 2
                self.gb += 1
                for hf in range(2):
                    wv, wk = self.stream_w(Wd, c0 + j * 128, 128, nh, k0=hf * nh)
                    for k in range(nh):
                        kk = hf * nh + k
                        self.mm(self.ps[b][:, 0:TT], wv[:, k, :], xn[:, kk, :], kk == 0, kk == nk - 1,
                                [wk, xkeys[kk]], [("ps", b)])
                evac(j, self.ps[b], ("ps", b))
            return
        while j < ntiles:
            nt = min(2, ntiles - j)
            wv, wk = self.stream_w(Wd, c0 + j * 128, nt * 128, nk)
            for jj in range(nt):
                b = self.gb % 2
                self.gb += 1
                for k in range(nk):
                    self.mm(self.ps[b][:, 0:TT], wv[:, k, jj * 128:(jj + 1) * 128], xn[:, k, :], k == 0, k == nk - 1,
                            [wk, xkeys[k]], [("ps", b)])
                evac(j + jj, self.ps[b], ("ps", b))
            j += nt

    def add_resid(self, Wd, rhs, nk, rkeys):
        def evac(j, ps, pk):
            self.tt("dve", self.h[:, j, :], self.h[:, j, :], ps[:, 0:TT], ALU.add, [("h", j), pk], [("h", j)])
        self.proj_fm(Wd, 0, KT, rhs, evac, nk=nk, xkeys=rkeys)

    def ffn(self, wcol, W1, W3, W2, es):
        xn = self.xn
        self.rmsnorm(wcol, xn)
        g = es.enter_context(self.sb("ffn_g", [128, HT, TT], BF16))
        tmp = es.enter_context(self.sb("ffn_tmp", [128, 2, TT], F32))
        for i in range(HT):
            wv, wk = self.stream_w(None, 0, 0, pieces=((W1, i * 128, 128), (W3, i * 128, 128)))
            for k in range(KT):
                self.mm(self.ps[0][:, 0:TT], wv[:, k, 0:128], xn[:, k, :], k == 0, k == KT - 1, [wk, ("xn", k)], [("ps", 0)])
            for k in range(KT):
                self.mm(self.ps[1][:, 0:TT], wv[:, k, 128:256], xn[:, k, :], k == 0, k == KT - 1, [wk, ("xn", k)], [("ps", 1)])
            self.act(tmp[:, i % 2, :], self.ps[0][:, 0:TT], AF.Silu, [("ps", 0)], [("ftmp", i % 2)])
            self.tt("dve", g[:, i, :], tmp[:, i % 2, :], self.ps[1][:, 0:TT], ALU.mult, [("ftmp", i % 2), ("ps", 1)], [("g", i)])
        self.add_resid(W2, g, HT, [("g", i) for i in range(HT)])

    def layer1_setup(self, es):
        nc = self.nc
        E = es.enter_context
        d = self.dram_in
        self.w_in1 = "w_in1p"
        self.w_out1 = "w_out1p"
        self.w1_1, self.w3_1, self.w2_1 = "ffn1_w1", "ffn1_w3", "ffn1_w2"
        p1 = d("params1", [128, 64])
        gw2 = d("gla_gate_w2", [16, 512])
        self.p1 = E(self.sb("p1", [128, 64], F32))
        self.load(self.p1[:], p1[:, :], ["params"])
        self.gw2_bf = E(self.sb("gw2_bf", [16, 512], BF16))
        self.load(self.gw2_bf[:], gw2[:, :], ["params"], cast=True)
        self.sinkE = E(self.sb("sinkE", [128, 8], F32))
        self.negb = E(self.sb("negb", [128, 4], F32))
        self.act(self.sinkE[:], self.p1[:, 34:42], AF.Exp, ["params"], ["params2"])
        self.ts("dve", self.negb[:], self.p1[:, 42:46], -1.0, None, ALU.mult, None, ["params"], ["params2"])
        self.kn_hist = E(self.sb("kn_hist", [128, 2, 128], BF16))
        self.Vpad = E(self.sb("Vpad", [128, 5, 4, 128], BF16))
        self.memset("dve", self.Vpad[:], 0.0, ["Vpad"])
        self.memset("dve", self.kn_hist[:], 0.0, ["knh"])
        self.Sf = E(self.sb("gla_Sf", [128, 4, 256], F32))
        self.Sb = E(self.sb("gla_Sb", [128, 4, 256], BF16))
        self.memset("dve", self.Sf[:], 0.0, [("Sf", i) for i in range(4)])
        self.memset("dve", self.Sb[:], 0.0, [("Sb", i) for i in range(4)])

    def layer1(self, ti):
        nc, ps = self.nc, self.ps
        p1 = self.p1
        with ExitStack() as es:
            E = es.enter_context
            xn = self.xn
            self.rmsnorm(p1[:, 0:16], xn)
            ymix = E(self.sb("ymix1", [128, 16, TT], BF16))
            qd = E(self.sb("qd", [128, 4, TT], BF16))
            kd = E(self.sb("kd", [128, 4, TT], BF16))
            sg = E(self.sb("sg", [128, 8, TT], BF16))
            glr = E(self.sb("glr", [16, TT], BF16))
            vd = E(self.sb("vd_tok", [128, NB, 1024], BF16))
            esA = ExitStack()
            EA = esA.enter_context
            qn = EA(self.sb("qn", [128, 10, 128 + TT], BF16))
            qkf = EA(self.sb("qkf", [128, 2, TT], F32))
            sq2 = EA(self.sb("sq2", [128, 2, TT], BF16))
            rs2 = EA(self.sb("rs2", [128, 2, TT], F32))
            self.cp("pool", qn[:, 8:10, 0:128], self.kn_hist[:], ["knh"], [("qn", 8), ("qn", 9)])

            def evac_qk(j, pst, pk):
                i = j % 2
                self.cp("act", qkf[:, i, :], pst[:, 0:TT], [pk], [("qkf", i)])
                self.act(sq2[:, i, :], pst[:, 0:TT], AF.Square, [pk], [("sq2", i)])
                self.mm(ps[6][:, 0:TT], self.bones_bf[:], sq2[:, i, :], True, True, [("sq2", i), "consts"], [("ps", 6)])
                self.act(rs2[:, i, :], ps[6][:, 0:TT], AF.Ln, [("ps", 6)], [("rs2", i)], scale=1.0 / 64, bias=self.eps_col[:, 0:1])
                self.act(rs2[:, i, :], rs2[:, i, :], AF.Exp, [("rs2", i)], [("rs2", i)], scale=-0.5)
                wc = p1[:, 32:33] if j < 8 else p1[:, 33:34]
                self.stt("dve", qn[:, j, 128:128 + TT], qkf[:, i, :], wc, rs2[:, i, :], ALU.mult, ALU.mult,
                         [("qkf", i), ("rs2", i), "params"], [("qn", j)])
            self.proj_fm(self.w_in1, 0, 10, xn, evac_qk)

            def evac_qd(j, pst, pk):
                self.act(qd[:, j, :], pst[:, 0:TT], AF.Copy, [pk], [("qd", j)], scale=128.0 ** -0.5)
            self.proj_fm(self.w_in1, 1280, 4, xn, evac_qd)

            def evac_kd(j, pst, pk):
                self.cp("act", kd[:, j, :], pst[:, 0:TT], [pk], [("kd", j)])
            self.proj_fm(self.w_in1, 1792, 4, xn, evac_kd)

            def evac_sg(j, pst, pk):
                self.act(sg[:, j, :], pst[:, 0:TT], AF.Silu, [pk], [("sg", j)])
            self.proj_fm(self.w_in1, 2304, 8, xn, evac_sg)
            wv, wk = self.stream_w(self.w_in1, 3328, 16)
            for k in range(KT):
                self.mm(ps[0][0:16, 0:TT], wv[:, k, :], xn[:, k, :], k == 0, k == KT - 1, [wk, ("xn", k)], [("ps", 0)])
            self.cp("act", glr[:], ps[0][0:16, 0:TT], [("ps", 0)], ["glr"])
            wv, wk = self.stream_w(self.w_in1, 3344, 256)
            for b in range(NB):
                for k in range(KT):
                    self.mm(ps[1][:, 0:256], xn[:, k, b * 128:(b + 1) * 128], wv[:, k, :], k == 0, k == KT - 1, [wk, ("xn", k)], [("ps", 1)])
                src = ps[1][:, 0:256].rearrange("p (h d) -> p h d", d=64)
                for e in range(2):
                    self.cp("act", self.Vpad[:, b + 1, e::2, 64 * e:64 * e + 64], src[:, e::2, :], [("ps", 1)], [("Vpad", b + 1)])
            for qr in range(4):
                wv, wk = self.stream_w(self.w_in1, 3600 + qr * 256, 256)
                for b in range(NB):
                    pb = self.gb % 2
                    self.gb += 1
                    for k in range(KT):
                        self.mm(ps[pb][:, 0:256], xn[:, k, b * 128:(b + 1) * 128], wv[:, k, :], k == 0, k == KT - 1, [wk, ("xn", k)], [("ps", pb)])
                    self.cp("act", vd[:, b, qr * 256:(qr + 1) * 256], ps[pb][:, 0:256], [("ps", pb)], [("vd", b, qr)])

            pT = EA(self.sb("pT", [128, 4, 512], BF16))
            den = EA(self.sb("den", [128, 512], F32))
            for j in range(2):
                for b in range(NB):
                    first = (ti == 0 and b == 0)
                    kbs = [1] if first else [0, 1]
                    combos = [(e, kb) for e in range(2) for kb in kbs]
                    for (e, kb) in combos:
                        sb = 2 + (e * 2 + kb) % 2
                        pi = e * 2 + kb
                        kc0 = b * 128 + kb * 128
                        self.mm(ps[sb][:, 0:512].rearrange("p (r q) -> p r q", q=128),
                                qn[64 * e:64 * e + 64, 8 + j, kc0:kc0 + 128],
                                qn[64 * e:64 * e + 64, 4 * j:4 * j + 4, 128 + b * 128:128 + (b + 1) * 128],
                                True, False, [("qn", 8 + j)] + [("qn", 4 * j + r) for r in range(4)], [("ps", sb)])
                        self.mm(ps[sb][:, 0:512], self.ident_bf[:], self.maskPC[:, kb, :], False, True, ["consts"], [("ps", sb)])
                        self.act(pT[:, pi, :], ps[sb][:, 0:512], AF.Exp, [("ps", sb)], [("pT", pi)], scale=0.125)
                    n = len(combos)
                    for i, (e, kb) in enumerate(combos):
                        pi = e * 2 + kb
                        self.mm(ps[4][:, 0:512], self.Vpad[:, b + kb, 2 * j + e, :], pT[:, pi, :], i == 0, i == n - 1,
                                [("Vpad", b + kb), ("pT", pi)], [("ps", 4)])
                    for i, (e, kb) in enumerate(combos):
                        pi = e * 2 + kb
                        self.mm(ps[5][:, 0:512], self.onespad[:, e, :], pT[:, pi, :], i == 0, i == n - 1,
                                ["consts", ("pT", pi)], [("ps", 5)])
                    self.tt("dve", den[:].rearrange("p (r q) -> p r q", q=128), ps[5][:, 0:512].rearrange("p (r q) -> p r q", q=128),
                            self.sinkE[:, 4 * j:4 * j + 4].unsqueeze(2).broadcast_to([128, 4, 128]), ALU.add,
                            [("ps", 5), "params2"], ["den"])
                    self.S.add("dve", lambda e_, den=den: e_.reciprocal(out=den[:], in_=den[:]), ["den"], ["den"])
                    self.tt("dve", ymix[:, 4 * j:4 * j + 4, b * 128:(b + 1) * 128],
                            ps[4][:, 0:512].rearrange("p (r q) -> p r q", q=128),
                            den[:].rearrange("p (r q) -> p r q", q=128), ALU.mult,
                            [("ps", 4), "den"], [("ymix", 4 * j + r) for r in range(4)])
            self.cp("pool", self.kn_hist[:], qn[:, 8:10, TT:TT + 128], [("qn", 8), ("qn", 9)], ["knh"])
            self.cp("pool", self.Vpad[:, 0, :, :], self.Vpad[:, NB, :, :], [("Vpad", NB)], [("Vpad", 0)])

            self.S.barrier()
            esA.close()
            ef = E(self.sb("gla_e", [128, TT], F32))
            bcp = E(self.sb("gla_bc", [128, TT], F32))
            Ep = E(self.sb("gla_Ep", [128, TT], F32))
            En = E(self.sb("gla_En", [128, TT], F32))
            qt = E(self.sb("gla_qt", [128, TT], BF16))
            kt_ = E(self.sb("gla_kt", [128, TT], BF16))
            ktok = E(self.sb("gla_ktok", [128, NB, 128], BF16))
            nbl = E(self.sb("gla_nb", [128, 8], F32))
            dec = E(self.sb("gla_dec", [128, 8], F32))
            attm = E(self.sb("gla_attm", [128, 2, 128], BF16))
            of = E(self.sb("gla_of", [128, 2, TT], F32))
            osq = E(self.sb("gla_osq", [128, 2, TT], BF16))
            for hd in range(4):
                self.mm(ps[0][:, 0:TT], self.gw2_bf[0:16, hd * 128:(hd + 1) * 128], glr[0:16, :], True, True, ["params", "glr"], [("ps", 0)])
                self.act(ef[:], ps[0][:, 0:TT], AF.Exp, [("ps", 0), "params2"], ["ef"], scale=-1.0, bias=self.negb[:, hd:hd + 1])
                self.act(ef[:], ef[:], AF.Ln, ["ef"], ["ef"], bias=self.one_col[:, 0:1])
                self.S.add("dve", lambda e_, bcp=bcp, ef=ef: e_.tensor_tensor_scan(out=bcp[:], data0=self.segm[:], data1=ef[:], initial=0.0, op0=ALU.mult, op1=ALU.add),
                           ["ef", "consts"], ["bcp"])
                self.act(Ep[:], bcp[:], AF.Exp, ["bcp"], ["Ep"], scale=-1.0 / 16)
                self.act(En[:], bcp[:], AF.Exp, ["bcp"], ["En"], scale=1.0 / 16)
                self.tt("dve", qt[:], qd[:, hd, :], Ep[:], ALU.mult, [("qd", hd), "Ep"], ["qt"])
                self.tt("dve", kt_[:], kd[:, hd, :], En[:], ALU.mult, [("kd", hd), "En"], ["kt"])
                self.act(dec[:], bcp[:, 63::64], AF.Exp, ["bcp"], ["dec"], scale=-1.0 / 16)
                for b in range(NB):
                    self.tr(self.psT[:, 0:128], kt_[:, b * 128:(b + 1) * 128], self.ident_bf[:], ["kt", "consts"], [("ps", 7)])
                    self.cp("act", ktok[:, b, :], self.psT[:, 0:128], [("ps", 7)], [("ktok", b)])
                for b in range(NB):
                    bs = slice(b * 128, (b + 1) * 128)
                    self.mm(ps[4][:, 0:128], kt_[:, bs], qt[:, bs], True, True, ["kt", "qt"], [("ps", 4)])
                    self.tt("dve", attm[:, b % 2, :], ps[4][:, 0:128], self.gmask[:], ALU.mult, [("ps", 4), "consts"], [("attm", b % 2)])
                    for c in range(2):
                        ci = b * 2 + c
                        cs = slice(b * 128 + c * 64, b * 128 + (c + 1) * 64)
                        for vt in range(2):
                            vcol = slice(hd * 256 + vt * 128, hd * 256 + (vt + 1) * 128)
                            self.mm(ps[2 + vt][:, cs], vd[:, b, vcol], attm[:, b % 2, c * 64:(c + 1) * 64], True, False,
                                    [("vd", b, hd), ("attm", b % 2)], [("ps", 2 + vt)])
                            self.mm(ps[2 + vt][:, cs], self.Sb[:, hd, vt * 128:(vt + 1) * 128], qt[:, cs], False, True,
                                    [("Sb", hd), "qt"], [("ps", 2 + vt)])
                        self.mm(ps[5][:, 0:256], ktok[64 * c:64 * c + 64, b, :], vd[64 * c:64 * c + 64, b, hd * 256:(hd + 1) * 256], True, True,
                                [("ktok", b), ("vd", b, hd)], [("ps", 5)])
                        self.tt("dve", self.Sf[:, hd, :], self.Sf[:, hd, :], ps[5][:, 0:256], ALU.add, [("Sf", hd), ("ps", 5)], [("Sf", hd)])
                        self.act(self.Sb[:, hd, :], self.Sf[:, hd, :], AF.Copy, [("Sf", hd), "dec"], [("Sb", hd)], scale=dec[:, ci:ci + 1])
                        self.act(self.Sf[:, hd, :], self.Sf[:, hd, :], AF.Copy, [("Sf", hd), "dec"], [("Sf", hd)], scale=dec[:, ci:ci + 1])
                for vt in range(2):
                    self.cp("act", of[:, vt, :], ps[2 + vt][:, 0:TT], [("ps", 2 + vt)], [("of", vt)])
                    self.act(osq[:, vt, :], ps[2 + vt][:, 0:TT], AF.Square, [("ps", 2 + vt)], [("osq", vt)])
                for vt in range(2):
                    self.mm(ps[6][:, 0:TT], self.ones_bf[:], osq[:, vt, :], vt == 0, vt == 1, [("osq", vt), "consts"], [("ps", 6)])
                self.act(ef[:], ps[6][:, 0:TT], AF.Ln, [("ps", 6)], ["ef"], scale=1.0 / 256, bias=self.eps_col[:, 0:1])
                self.act(ef[:], ef[:], AF.Exp, ["ef"], ["ef"], scale=-0.5)
                for vt in range(2):
                    self.stt("dve", of[:, vt, :], of[:, vt, :], p1[:, 46 + vt:47 + vt], ef[:], ALU.mult, ALU.mult,
                             [("of", vt), "ef", "params"], [("of", vt)])
                    self.tt("dve", ymix[:, 8 + hd * 2 + vt, :], of[:, vt, :], sg[:, hd * 2 + vt, :], ALU.mult,
                            [("of", vt), ("sg", hd * 2 + vt)], [("ymix", 8 + hd * 2 + vt)])
            self.add_resid(self.w_out1, ymix, KT, [("ymix", i) for i in range(KT)])
        self.S.barrier()
        with ExitStack() as es:
            self.ffn(p1[:, 16:32], self.w1_1, self.w3_1, self.w2_1, es)
        self.S.barrier()

    def sin_of(self, out, x, shift, tmp, tmpi, key):
        c = (shift + 5 * PI) / (2 * PI) - 0.5
        self.ts("dve", tmp, x, 1.0 / (2 * PI), c, ALU.mult, ALU.add, [key], [key + "_t"])
        self.cp("dve", tmpi, tmp, [key + "_t"], [key + "_i"])
        self.cp("dve", tmp, tmpi, [key + "_i"], [key + "_t"])
        self.stt("dve", tmp, tmp, -2 * PI, x, ALU.mult, ALU.add, [key + "_t", key], [key + "_t"])
        self.ts("dve", tmp, tmp, shift + 4 * PI, None, ALU.add, None, [key + "_t"], [key + "_t"])
        self.ts("dve", tmp, tmp, -PI, PI, ALU.max, ALU.min, [key + "_t"], [key + "_t"])
        self.act(out, tmp, AF.Sin, [key + "_t"], [key + "_o"])

    def layer0_setup(self, es):
        nc = self.nc
        E = es.enter_context
        d = self.dram_in
        cf = self.cf
        self.sgn = cf[:, 642:643]
        self.m4 = cf[:, 643:647]
        self.TriF = cf[:, 648:776]
        self.UstrF = cf[:, 776:904]
        self.negmF = cf[:, 904:1032]
        self.ident_f = cf[:, 1032:1160]
        self.ones_f = cf[:, 1160:1288]
        self.swap_f = cf[:, 1288:1416]
        self.tpos = cf[:, 1416:1544]
        self.w_in0 = "w_in0p"
        self.w_out0 = "w_out0"
        self.w1_0, self.w3_0, self.w2_0 = "ffn0_w1", "ffn0_w3", "ffn0_w2"
        p0d = d("params0", [128, 320])
        s5ch_d = d("s5ch", [128, 5, 256])
        s5C_d = d("s5C", [128, 32, 2, 16])
        nw_d = d("ssd_nw", [128, 1536])
        glu_d = d("s5_glu_w", [512, 512])
        p0 = self.p0 = E(self.sb("p0", [128, 320], F32))
        self.load(p0[:], p0d[:, :], ["params"])
        self.Cc = E(self.sb("s5Cc", [128, 32, 2, 16], BF16))
        self.load(self.Cc[:], s5C_d[:, :, :, :], ["params"], cast=True)
        self.nw = E(self.sb("ssdnw", [128, 1536], BF16))
        self.load(self.nw[:], nw_d[:, :], ["params"], cast=True)
        self.gluw = E(self.sb("gluw", [128, 4, 512], BF16))
        self.load(self.gluw[:], glu_d.rearrange("(k p) n -> p k n", p=128), ["params"], cast=True)
        self.COS = E(self.sb("COS", [128, 32, 128], BF16))
        self.SIN = E(self.sb("SIN", [128, 32, 128], BF16))
        self.BAc = E(self.sb("BAc", [128, 4, 4, 128], BF16))
        self.BBc = E(self.sb("BBc", [128, 4, 4, 128], BF16))
        self.diagD = E(self.sb("diagD", [128, 4, 128], BF16))
        self.rho = E(self.sb("rho", [128, 32], F32))
        self.CS2 = E(self.sb("CS2", [128, 32, 2], F32))
        self.carry = E(self.sb("carry", [128, 32], F32))
        self.STf = E(self.sb("STf", [128, 24, 64], F32))
        self.STb = E(self.sb("STb", [128, 24, 64], BF16))
        self.chist = E(self.sb("chist", [128, 20, 3], BF16))
        self.Arow = E(self.sb("Arow", [128, 24], F32))
        self.zeros_bf = E(self.sb("zeros_bf", [128, 512], BF16))
        self.memset("dve", self.zeros_bf[:], 0.0, ["zeros"])
        self.memset("dve", self.carry[:], 0.0, ["carry"])
        self.memset("dve", self.STf[:], 0.0, ["STf"])
        self.memset("dve", self.STb[:], 0.0, ["STb"])
        self.memset("dve", self.chist[:], 0.0, ["chist"])
        self.act(self.Arow[:], p0[:, 140:164], AF.Exp, ["params"], ["Arow"])
        self.ts("dve", self.Arow[:], self.Arow[:], -1.0, None, ALU.mult, None, ["Arow"], ["Arow"])
        for j in range(4):
            self.ts("dve", self.diagD[:, j, :], self.ident_f, p0[:, 32 + j:33 + j], None, ALU.mult, None, ["params", "consts"], ["diagD"])
        with ExitStack() as tes:
            TE = tes.enter_context
            ang = TE(self.sb("ang", [128, 32, 128], F32))
            tmp = TE(self.sb("sc_t", [128, 4096], F32))
            tmpi = TE(self.sb("sc_i", [128, 4096], I32))
            sm = TE(self.sb("s5sm", [128, 6, 32], F32))
            dtst, th, lam, angq, cq, sq_ = (sm[:, i, :] for i in range(6))
            self.act(dtst, p0[:, 252:284], AF.Exp, ["params"], ["dtst"])
            self.tt("dve", th, p0[:, 220:252], dtst, ALU.mult, ["params", "dtst"], ["th"])
            self.tt("dve", lam, p0[:, 188:220], dtst, ALU.mult, ["params", "dtst"], ["lam"])
            self.act(self.rho[:], lam, AF.Exp, ["lam"], ["rho"])
            self.tt("dve", ang[:], th.unsqueeze(2).broadcast_to([128, 32, 128]), self.tpos.unsqueeze(1).broadcast_to([128, 32, 128]), ALU.mult,
                    ["th", "consts"], ["ang"])
            angf = ang[:].rearrange("p g t -> p (g t)")
            self.sin_of(self.COS[:].rearrange("p g t -> p (g t)"), angf, PI / 2, tmp[:], tmpi[:], "ang")
            self.sin_of(self.SIN[:].rearrange("p g t -> p (g t)"), angf, 0.0, tmp[:], tmpi[:], "ang")
            self.ts("dve", angq, th, 128.0, None, ALU.mult, None, ["th"], ["angq"])
            self.sin_of(cq, angq, PI / 2, tmp[:, 0:32], tmpi[:, 0:32], "angq")
            self.cp("dve", self.CS2[:, :, 0], cq, ["angq_o"], ["CS2a"])
            self.sin_of(sq_, angq, 0.0, tmp[:, 0:32], tmpi[:, 0:32], "angq")
            self.ts("dve", self.CS2[:, :, 1], sq_, self.sgn, None, ALU.mult, None, ["angq_o", "consts"], ["CS2b"])
            self.S.barrier(full=True)
        with ExitStack() as tes:
            TE = tes.enter_context
            tmp = TE(self.sb("sc_t2", [128, 256], F32))
            tmpi = TE(self.sb("sc_i2", [128, 256], I32))
            ch = TE(self.sb("s5ch", [128, 5, 256], F32))
            self.load(ch[:], s5ch_d[:, :, :], ["ch"])
            w = TE(self.sb("s5w", [128, 14, 256], F32))
            Are, Aim, ldt, BreT, BimT = (ch[:, i, :] for i in range(5))
            dt, lr, thc, mag, cs, sn, abr, abi, er, den, cr, ci, u1, u2 = (w[:, i, :] for i in range(14))
            K = "chw"
            self.act(dt, ldt, AF.Exp, ["ch"], [K])
            self.tt("dve", lr, Are, dt, ALU.mult, ["ch", K], [K])
            self.tt("dve", thc, Aim, dt, ALU.mult, ["ch", K], ["thc"])
            self.act(mag, lr, AF.Exp, [K], [K])
            self.sin_of(cs, thc, PI / 2, tmp[:, 0:256], tmpi[:, 0:256], "thc")
            self.tt("dve", abr, mag, cs, ALU.mult, [K, "thc_o"], [K])
            self.sin_of(sn, thc, 0.0, tmp[:, 0:256], tmpi[:, 0:256], "thc")
            self.tt("dve", abi, mag, sn, ALU.mult, [K, "thc_o"], [K])
            self.ts("dve", er, abr, -1.0, None, ALU.add, None, [K], [K])
            self.tt("dve", den, Are, Are, ALU.mult, ["ch", K], [K])
            self.tt("dve", u1, Aim, Aim, ALU.mult, ["ch", K], [K])
            self.tt("dve", den, den, u1, ALU.add, [K], [K])
            self.S.add("dve", lambda e_: e_.reciprocal(out=den, in_=den), [K], [K])
            self.tt("dve", u1, er, Are, ALU.mult, ["ch", K], [K])
            self.tt("dve", u2, abi, Aim, ALU.mult, ["ch", K], [K])
            self.tt("dve", u1, u1, u2, ALU.add, [K], [K])
            self.tt("dve", cr, u1, den, ALU.mult, [K], [K])
            self.tt("dve", u1, abi, Are, ALU.mult, ["ch", K], [K])
            self.tt("dve", u2, er, Aim, ALU.mult, ["ch", K], [K])
            self.tt("dve", u1, u1, u2, ALU.subtract, [K], [K])
            self.tt("dve", ci, u1, den, ALU.mult, [K], [K])
            bf = TE(self.sb("s5bf", [128, 2, 4, 128], F32))
            v4 = lambda ap: ap.rearrange("p (j n) -> p j n", n=64)
            self.tt("dve", u1, BreT, cr, ALU.mult, ["ch", K], [K])
            self.tt("dve", u2, BimT, ci, ALU.mult, ["ch", K], [K])
            self.tt("dve", bf[:, 0, :, 0:64], v4(u1), v4(u2), ALU.subtract, [K], ["bf"])
            self.cp("dve", bf[:, 1, :, 64:128], bf[:, 0, :, 0:64], ["bf"], ["bf"])
            self.tt("dve", u1, BreT, ci, ALU.mult, ["ch", K], [K])
            self.tt("dve", u2, BimT, cr, ALU.mult, ["ch", K], [K])
            self.tt("dve", bf[:, 0, :, 64:128], v4(u1), v4(u2), ALU.add, [K], ["bf"])
            self.cp("dve", bf[:, 1, :, 0:64], bf[:, 0, :, 64:128], ["bf"], ["bf"])
            for q in range(4):
                self.ts("dve", self.BAc[:, :, q, :], bf[:, 0, :, :], self.m4[:, q:q + 1], None, ALU.mult, None, ["bf", "consts"], ["BAc"])
                self.ts("dve", self.BBc[:, :, q, :], bf[:, 1, :, :], self.m4[:, q:q + 1], None, ALU.mult, None, ["bf", "consts"], ["BAc"])
            self.S.barrier(full=True)

    def layer0(self, ti):
        nc, ps = self.nc, self.ps
        p0 = self.p0
        xn = self.xn
        with ExitStack() as es:
            E = es.enter_context
            self.rmsnorm(p0[:, 0:16], xn)
            ymix = E(self.sb("ymix0a", [128, 4, TT], BF16))
            esA = ExitStack()
            EA = esA.enter_context
            u_bf = EA(self.sb("u_bf", [128, 4, TT], BF16))
            t1 = EA(self.sb("s5t1", [128, 2, TT], F32))
            t2 = EA(self.sb("s5t2", [128, 2, TT], F32))
            xp = EA(self.sb("s5xp", [128, 4, TT], F32))
            gs = EA(self.sb("s5gs", [128, 4, TT], F32))
            P12 = EA(self.sb("s5P", [128, 2, 2, TT], BF16))
            v2 = EA(self.sb("s5v2", [128, 4, 2], F32))
            ygT = EA(self.sb("ygT", [128, 4, TT], BF16))

            def evac_u(j, pst, pk):
                self.cp("act", u_bf[:, j, :], pst[:, 0:TT], [pk], [("u", j)])
            self.proj_fm(self.w_in0, 0, 4, xn, evac_u)
            for b in range(NB):
                self.mm(ps[b][:, 0:512], self.ident_bf, self.zeros_bf[:], True, False, ["consts", "zeros"], [("ps", b)])
                for j in range(4):
                    self.mm(ps[b][:, j * 128:(j + 1) * 128], u_bf[:, j, b * 128:(b + 1) * 128], self.diagD[:, j, :], False, False,
                            [("u", j), "diagD"], [("ps", b)])
            v3 = lambda ap: ap.rearrange("p (c t) -> p c t", t=128)
            CB = (4, 5, 6)
            for gset in range(8):
                for i in range(4):
                    g = gset * 4 + i
                    j, gp = g // 8, g % 8
                    hf, q = gp // 4, gp % 4
                    i2 = i % 2
                    rows = slice(64 * hf, 64 * hf + 64)
                    pa, pb = 4, 5
                    self.mm(ps[pa][:, 0:TT], self.BAc[rows, j, q, :], u_bf[rows, j, :], True, True, [("u", j), "BAc"], [("ps", pa)])
                    self.mm(ps[pb][:, 0:TT], self.BBc[rows, j, q, :], u_bf[rows, j, :], True, True, [("u", j), "BAc"], [("ps", pb)])
                    cosb = self.COS[:, g, :].unsqueeze(1).broadcast_to([128, NB, 128])
                    sinb = self.SIN[:, g, :].unsqueeze(1).broadcast_to([128, NB, 128])
                    self.tt("dve", v3(t1[:, i2, :]), v3(ps[pa][:, 0:TT]), cosb, ALU.mult, [("ps", pa), "trig"], [("t1", i2)])
                    self.stt("dve", v3(t2[:, i2, :]), v3(ps[pb][:, 0:TT]), self.sgn, sinb, ALU.mult, ALU.mult, [("ps", pb), "trig", "consts"], [("t2", i2)])
                    self.tt("pool", xp[:, i, :], t1[:, i2, :], t2[:, i2, :], ALU.add, [("t1", i2), ("t2", i2)], [("xp", i)])
                for c in range(NB):
                    cs_ = slice(c * 128, (c + 1) * 128)
                    last = c * 128 + 127
                    for i in range(4):
                        g = gset * 4 + i
                        cb_ = CB[i % 3]
                        self.S.add("dve", lambda e_, g=g, i=i, cs_=cs_: e_.tensor_tensor_scan(
                            out=gs[:, i, cs_], data0=self.rho[:, g:g + 1].broadcast_to([128, 128]), data1=xp[:, i, cs_],
                            initial=self.carry[:, g:g + 1], op0=ALU.mult, op1=ALU.add),
                            [("xp", i), ("carry", g), "rho"], [("gs", i)])
                        self.tt("pool", v2[:, i, :], gs[:, i, last:last + 1].broadcast_to([128, 2]), self.CS2[:, g, :], ALU.mult,
                                [("gs", i), "CS2"], [("v2", i)])
                        self.mm(ps[cb_][:, g:g + 1], self.ident_f, v2[:, i, 0:1], True, False, [("v2", i), "consts"], [("ps", cb_)])
                        self.mm(ps[cb_][:, g:g + 1], self.swap_f, v2[:, i, 1:2], False, True, [("v2", i), "consts"], [("ps", cb_)])
                        self.cp("act", self.carry[:, g:g + 1], ps[cb_][:, g:g + 1], [("ps", cb_)], [("carry", g)])
                for i in range(4):
                    g = gset * 4 + i
                    i2 = i % 2
                    cosb = self.COS[:, g, :].unsqueeze(1).broadcast_to([128, NB, 128])
                    sinb = self.SIN[:, g, :].unsqueeze(1).broadcast_to([128, NB, 128])
                    self.stt("dve", v3(P12[:, i2, 0, :]), v3(gs[:, i, :]), self.sgn, cosb, ALU.mult, ALU.mult, [("gs", i), "trig", "consts"], [("P", i2)])
                    self.stt("dve", v3(P12[:, i2, 1, :]), v3(gs[:, i, :]), -1.0, sinb, ALU.mult, ALU.mult, [("gs", i), "trig"], [("P", i2)])
                    for b in range(NB):
                        bs = slice(b * 128, (b + 1) * 128)
                        self.mm(ps[b][:, 16 * g:16 * g + 16], P12[:, i2, 0, bs], self.Cc[:, g, 0, :], False, False, [("P", i2), "params"], [("ps", b)])
                        self.mm(ps[b][:, 16 * g:16 * g + 16], P12[:, i2, 1, bs], self.Cc[:, g, 1, :], False, True, [("P", i2), "params"], [("ps", b)])
            yf = EA(self.sb("s5yf", [128, 2, 512], F32))
            yt = EA(self.sb("s5yt", [128, 2, 512], F32))
            ygt = EA(self.sb("s5ygt", [128, 2, 512], BF16))
            GC = float(2.0 * np.sqrt(2.0 / np.pi))
            for b in range(NB):
                i2 = b % 2
                self.cp("act", yf[:, i2, :], ps[b][:, 0:512], [("ps", b)], [("yf", i2)])
                self.tt("pool", yt[:, i2, :], yf[:, i2, :], yf[:, i2, :], ALU.mult, [("yf", i2)], [("yt", i2)])
                self.ts("dve", yt[:, i2, :], yt[:, i2, :], 0.044715, 1.0, ALU.mult, ALU.add, [("yt", i2)], [("yt", i2)])
                self.tt("pool", yt[:, i2, :], yt[:, i2, :], yf[:, i2, :], ALU.mult, [("yt", i2), ("yf", i2)], [("yt", i2)])
                self.act(yt[:, i2, :], yt[:, i2, :], AF.Sigmoid, [("yt", i2)], [("yt", i2)], scale=GC)
                self.tt("dve", ygt[:, i2, :], yf[:, i2, :], yt[:, i2, :], ALU.mult, [("yf", i2), ("yt", i2)], [("ygt", i2)])
                for k in range(4):
                    self.tr(self.psT[:, k * 128:(k + 1) * 128], ygt[:, i2, k * 128:(k + 1) * 128], self.ident_bf, [("ygt", i2), "consts"], [("ps", 7)])
                self.cp("act", ygT[:, :, b * 128:(b + 1) * 128], self.psT[:, 0:512].rearrange("p (k t) -> p k t", t=128), [("ps", 7)], ["ygT"])
            for jo in range(4):
                pb = 4 + jo % 2
                for k in range(4):
                    self.mm(ps[pb][:, 0:TT], self.gluw[:, k, jo * 128:(jo + 1) * 128], ygT[:, k, :], k == 0, k == 3, ["params", "ygT"], [("ps", pb)])
                self.act(yf[:, jo % 2, :], ps[pb][:, 0:TT], AF.Sigmoid, [("ps", pb), "params"], [("yf", jo % 2)], bias=p0[:, 36 + jo:37 + jo])
                self.tt("dve", ymix[:, jo, :], ygT[:, jo, :], yf[:, jo % 2, :], ALU.mult, ["ygT", ("yf", jo % 2)], [("ymix", jo)])
            self.S.barrier()
            esA.close()
            BC = E(self.sb("BC_fm", [128, 8, TT], BF16))
            xs_tok = E(self.sb("xs_tok", [128, NB, 1536], BF16))
            B_tok = E(self.sb("B_tok", [128, NB, 512], BF16))
            esB = ExitStack()
            EB = esB.enter_context
            xpre = EB(self.sb("xbc_pre", [128, 20, 3 + TT], BF16))
            acc = EB(self.sb("cacc", [128, 2, TT], F32))
            xsf = EB(self.sb("xs_fm", [128, 2, TT], BF16))
            self.cp("pool", xpre[:, :, 0:3], self.chist[:], ["chist"], [("xpre", ct) for ct in range(20)])

            def evac_x(j, pst, pk):
                self.cp("act", xpre[:, j, 3:3 + TT], pst[:, 0:TT], [pk], [("xpre", j)])
            self.proj_fm(self.w_in0, 512, 20, xn, evac_x)
            for ct in range(20):
                i2 = ct % 2
                eng = "dve"
                cw = lambda k: p0[:, 60 + ct * 4 + k:61 + ct * 4 + k]
                self.ts(eng, acc[:, i2, :], xpre[:, ct, 3:3 + TT], cw(3), None, ALU.mult, None, [("xpre", ct), "params"], [("acc", i2)])
                for k in range(3):
                    self.stt(eng, acc[:, i2, :], xpre[:, ct, k:k + TT], cw(k), acc[:, i2, :], ALU.mult, ALU.add, [("xpre", ct), ("acc", i2), "params"], [("acc", i2)])
                if ct < 12:
                    self.act(xsf[:, i2, :], acc[:, i2, :], AF.Silu, [("acc", i2), "params"], [("xsf", i2)], bias=p0[:, 40 + ct:41 + ct])
                    src = xsf[:, i2, :]
                    sk = ("xsf", i2)
                else:
                    self.act(BC[:, ct - 12, :], acc[:, i2, :], AF.Silu, [("acc", i2), "params"], [("BC", ct - 12)], bias=p0[:, 40 + ct:41 + ct])
                    src = BC[:, ct - 12, :]
                    sk = ("BC", ct - 12)
                if ct < 16:
                    for b in range(NB):
                        self.tr(self.psT[:, b * 128:(b + 1) * 128], src[:, b * 128:(b + 1) * 128], self.ident_bf, [sk, "consts"], [("ps", 7)])
                    dst = xs_tok[:, :, ct * 128:(ct + 1) * 128] if ct < 12 else B_tok[:, :, (ct - 12) * 128:(ct - 11) * 128]
                    self.cp("act", dst, self.psT[:, 0:512].rearrange("p (b t) -> p b t", t=128), [("ps", 7)], [("xstok", ct)])
            self.cp("pool", self.chist[:], xpre[:, :, TT:TT + 3], [("xpre", ct) for ct in range(20)], ["chist"])
            self.S.barrier()
            esB.close()
            ymixB = E(self.sb("ymix0b", [128, 12, TT], BF16))
            sz = E(self.sb("sz_tok", [128, NB, 1536], BF16))
            dtt = E(self.sb("dt_tok", [128, NB, 24], F32))
            dtA = E(self.sb("dtA", [128, NB, 24], F32))
            for cb in range(6):
                wv, wk = self.stream_w(self.w_in0, 3072 + cb * 256, 256)
                for b in range(NB):
                    pb = self.gb % 2
                    self.gb += 1
                    for k in range(KT):
                        self.mm(ps[pb][:, 0:256], xn[:, k, b * 128:(b + 1) * 128], wv[:, k, :], k == 0, k == KT - 1, [wk, ("xn", k)], [("ps", pb)])
                    self.act(sz[:, b, cb * 256:(cb + 1) * 256], ps[pb][:, 0:256], AF.Silu, [("ps", pb)], [("sz", b)])
            wv, wk = self.stream_w(self.w_in0, 4608, 24)
            for b in range(NB):
                pb = self.gb % 2
                self.gb += 1
                for k in range(KT):
                    self.mm(ps[pb][:, 0:24], xn[:, k, b * 128:(b + 1) * 128], wv[:, k, :], k == 0, k == KT - 1, [wk, ("xn", k)], [("ps", pb)])
                self.tt("dve", dtt[:, b, :], ps[pb][:, 0:24], p0[:, 284:308], ALU.add, [("ps", pb), "params"], ["dtt"])
            self.act(dtt[:], dtt[:], AF.Exp, ["dtt"], ["dtt"])
            self.act(dtt[:], dtt[:], AF.Ln, ["dtt"], ["dtt"], bias=self.one_col)
            self.tt("dve", dtA[:], dtt[:], self.Arow[:].unsqueeze(1).broadcast_to([128, NB, 24]), ALU.mult, ["dtt", "Arow"], ["dtA"])
            cbs = E(self.sb("cb_sb", [128, 2, 128], BF16))
            rhsD = E(self.sb("rhsD", [128, 6, 128], F32))
            LE = E(self.sb("LE", [128, 6, 256], F32))
            MT = E(self.sb("MT", [128, 6, 128], BF16))
            CTs = E(self.sb("CTs", [128, 6, 128], BF16))
            xdt = E(self.sb("xdt", [128, 6, 2, 64], BF16))
            yg = E(self.sb("ssd_yg", [128, 2, 384], F32))
            ss = E(self.sb("ssd_ss", [128, 4], F32))
            yn = E(self.sb("ssd_yn", [128, 2, 384], BF16))
            hc = 0
            for b in range(NB):
                bs = slice(b * 128, (b + 1) * 128)
                for g in range(4):
                    gi = g % 2
                    self.mm(ps[4][:, 0:128], BC[:, g, bs], BC[:, 4 + g, bs], True, True, [("BC", g), ("BC", 4 + g)], [("ps", 4)])
                    self.cp("act", cbs[:, gi, :], ps[4][:, 0:128], [("ps", 4)], [("cbs", gi)])
                    DB = (0, 1, 5)
                    ybank = 2 + gi
                    hs = list(range(6))
                    for r in hs:
                        hh = g * 6 + r
                        self.ts("pool", rhsD[:, r, :], self.TriF, dtA[:, b, hh:hh + 1], None, ALU.mult, None, ["dtA", "consts"], [("rhsD", r)])
                    for r in hs:
                        db = DB[r // 2]
                        o = (r % 2) * 256
                        self.mm(ps[db][:, o:o + 128], self.UstrF, rhsD[:, r, :], True, False, [("rhsD", r), "consts"], [("ps", db)])
                        self.mm(ps[db][:, o:o + 128], self.ident_f, self.negmF, False, True, ["consts"], [("ps", db)])
                        self.mm(ps[db][:, o + 128:o + 256], self.ones_f, rhsD[:, r, :], True, True, [("rhsD", r), "consts"], [("ps", db)])
                    for r in hs:
                        o = (r % 2) * 256
                        self.act(LE[:, r, :], ps[DB[r // 2]][:, o:o + 256], AF.Exp, [("ps", DB[r // 2])], [("LE", r)])
                    for r in hs:
                        hh = g * 6 + r
                        xsl = xs_tok[:, b, hh * 64:(hh + 1) * 64]
                        self.tt("dve", MT[:, r, :], cbs[:, gi, :], LE[:, r, 0:128], ALU.mult, [("cbs", gi), ("LE", r)], [("MT", r)])
                        self.ts("pool", xdt[:, r, 0, :], xsl, dtt[:, b, hh:hh + 1], None, ALU.mult, None, [("xstok", hh // 2), "dtt"], [("xdt", r)])
                        self.ts("pool", xdt[:, r, 1, :], xdt[:, r, 0, :], LE[:, r, 127:128], None, ALU.mult, None, [("xdt", r), ("LE", r)], [("xdtw", r)])
                        self.tt("dve", CTs[:, r, :], BC[:, 4 + g, bs], LE[:, r, 128:256], ALU.mult, [("BC", 4 + g), ("LE", r)], [("CTs", r)])
                    for r in hs:
                        hh = g * 6 + r
                        yc = slice(r * 64, (r + 1) * 64)
                        self.mm(ps[ybank][:, yc], MT[:, r, :], xdt[:, r, 0, :], True, False, [("MT", r), ("xdt", r)], [("ps", ybank)])
                        self.mm(ps[ybank][:, yc], CTs[:, r, :], self.STb[:, hh, :], False, True, [("CTs", r), ("STb", hh)], [("ps", ybank)])
                        self.mm(ps[6][:, yc], B_tok[:, b, g * 128:(g + 1) * 128], xdt[:, r, 1, :], True, True, [("xstok", 12 + g), ("xdtw", r)], [("ps", 6)])
                    for r in hs:
                        hh = g * 6 + r
                        yc = slice(r * 64, (r + 1) * 64)
                        xsl = xs_tok[:, b, hh * 64:(hh + 1) * 64]
                        self.stt("dve", self.STf[:, hh, :], self.STf[:, hh, :], LE[:, r, 255:256], ps[6][:, yc], ALU.mult, ALU.add,
                                 [("STf", hh), ("LE", r), ("ps", 6)], [("STf", hh)])
                        self.cp("act", self.STb[:, hh, :], self.STf[:, hh, :], [("STf", hh)], [("STb", hh)])
                        self.stt("dve", yg[:, gi, yc], xsl, p0[:, 164 + hh:165 + hh], ps[ybank][:, yc], ALU.mult, ALU.add,
                                 [("xstok", hh // 2), "params", ("ps", ybank)], [("yg", gi)])
                    gsl = slice(g * 384, (g + 1) * 384)
                    self.tt("dve", yg[:, gi, :], yg[:, gi, :], sz[:, b, gsl], ALU.mult, [("yg", gi), ("sz", b)], [("yg", gi)])
                    self.act(yn[:, gi, :], yg[:, gi, :], AF.Square, [("yg", gi)], [("yn", gi)])
                    self.S.add("dve", lambda e_, g=g, gi=gi: e_.reduce_sum(out=ss[:, g:g + 1], in_=yn[:, gi, :], axis=mybir.AxisListType.X), [("yn", gi)], [("ss", g)])
                    self.act(ss[:, g:g + 1], ss[:, g:g + 1], AF.Ln, [("ss", g)], [("ss", g)], scale=1.0 / 384, bias=self.eps_col)
                    self.act(ss[:, g:g + 1], ss[:, g:g + 1], AF.Exp, [("ss", g)], [("ss", g)], scale=-0.5)
                    self.stt("dve", yn[:, gi, :], yg[:, gi, :], ss[:, g:g + 1], self.nw[:, gsl], ALU.mult, ALU.mult, [("yg", gi), ("ss", g), "params"], [("yn", gi)])
                    for i in range(3):
                        self.tr(self.psT[:, i * 128:(i + 1) * 128], yn[:, gi, i * 128:(i + 1) * 128], self.ident_bf, [("yn", gi), "consts"], [("ps", 7)])
                    self.cp("act", ymixB[:, 3 * g:3 * g + 3, bs], self.psT[:, 0:384].rearrange("p (k t) -> p k t", t=128),
                            [("ps", 7)], [("ymix", 4 + 3 * g + i) for i in range(3)])
            if self.dbg == "ymix0":
                self.dma("pool", self.dbg_d[:, 0:4, ti * TT:(ti + 1) * TT], ymix[:], [("ymix", i) for i in range(KT)], ["dbgout"])
                self.dma("pool", self.dbg_d[:, 4:16, ti * TT:(ti + 1) * TT], ymixB[:], [("ymix", i) for i in range(KT)], ["dbgout2"])
            self.add_resid(self.w_out0, [ymix[:, i, :] for i in range(4)] + [ymixB[:, i, :] for i in range(12)], KT, [("ymix", i) for i in range(KT)])
        self.S.barrier()
        with ExitStack() as es:
            self.ffn(p0[:, 16:32], self.w1_0, self.w3_0, self.w2_0, es)
        self.S.barrier()

    def build(self):
        nc = self.nc
        T = self.T
        xT = self.dram_in("xT", [D, T])
        outT = nc.dram_tensor("outT", [D, T], F32, kind="ExternalOutput").ap()
        cst = self.dram_in("consts", [128, 3208])
        self.wpack = self.dram_in("wpack", [128, WTOT])
        self.wscr = nc.dram_tensor("wscr", [128, WTOT], BF16, kind="Internal").ap()
        if self.dbg:
            self.dbg_d = nc.dram_tensor("dbg", [128, 16, T], F32, kind="ExternalOutput").ap()
        with ExitStack() as es:
            E = es.enter_context
            self.h = E(self.sb("h", [128, KT, TT], F32))
            self.xn = E(self.sb("xn", [128, KT, TT], BF16))
            self.sq = E(self.sb("sq", [128, 2, TT], BF16))
            self.rstd = E(self.sb("rstd", [128, TT], F32))
            self.wbuf = [E(self.sb("wbuf%d" % i, [128, 4096], BF16)) for i in range(2)]
            self.ps = [E(nc.psum_tensor("ps%d" % i, [128, 512], F32)) for i in range(7)]
            self.psT = E(nc.psum_tensor("psT", [128, 1024], BF16))
            self.gb = 0
            cbf = E(self.sb("cbf", [128, 1664], BF16))
            self.load(cbf[:], cst[:, 0:1664], ["consts"], cast=True)
            self.ident_bf = cbf[:, 0:128]
            self.ones_bf = cbf[:, 128:256]
            self.bones_bf = cbf[:, 256:384]
            self.onespad = cbf[:, 384:640].rearrange("p (e m) -> p e m", m=128)
            self.maskPC = cbf[:, 640:1664].rearrange("p (k m) -> p k m", m=512)
            cf = E(self.sb("cf", [128, 1544], F32))
            self.load(cf[:], cst[:, 1664:3208], ["consts"])
            self.gmask = cf[:, 0:128]
            self.segm = cf[:, 128:640]
            self.eps_col = cf[:, 640:641]
            self.one_col = cf[:, 641:642]
            self.cf = cf
            if 1 in self.layers:
                self.layer1_setup(es)
            if 0 in self.layers:
                self.layer0_setup(es)
            self.S.barrier(full=True)
            for ti in range(self.NT):
                t0 = ti * TT
                for k in range(KT):
                    self.dma("sp", self.h[:, k, :], xT[k * 128:(k + 1) * 128, t0:t0 + TT], (), [("h", k)])
                if 0 in self.layers:
                    self.layer0(ti)
                if 1 in self.layers:
                    self.layer1(ti)
                outs = []
                for k in range(KT):
                    outs.append(self.S.add("sp", (lambda e, k=k, t0=t0: e.dma_start(out=outT[k * 128:(k + 1) * 128, t0:t0 + TT], in_=self.h[:, k, :])),
                                           [("h", k)], [("out", ti, k)], dma=True))
                self.S.barrier(full=(ti == self.NT - 1))
            self.S.emit(nc, es)
        return nc


def make_consts():
    c = np.zeros((128, 3208), np.float32)
    p = np.arange(128)
    c[:, 0:128] = np.eye(128)
    c[:, 128:256] = 1.0
    c[:, 256:384] = (p[:, None] // 64 == p[None, :] // 64)
    for e in range(2):
        c[:, 384 + e * 128 + 64 * e:384 + e * 128 + 64 * e + 64] = 1.0
    k = p[:, None]
    q = p[None, :]
    mP = np.where(k > q, 0.0, -30000.0)
    mC = np.where(k <= q, 0.0, -30000.0)
    c[:, 640:1152] = np.tile(mP, (1, 4))
    c[:, 1152:1664] = np.tile(mC, (1, 4))
    o = 1664
    s = p[:, None]
    l = p[None, :]
    c[:, o:o + 128] = ((s // 64 == l // 64) & (l >= s))
    t = np.arange(512)
    c[:, o + 128:o + 640] = (t % 64 != 0)[None, :]
    c[:, o + 640] = EPS
    c[:, o + 641] = 1.0
    c[:, o + 642] = np.where(p < 64, 1.0, -1.0)
    for q_ in range(4):
        c[:, o + 643 + q_] = ((p // 16) % 4 == q_)
    c[:, o + 648:o + 776] = (s <= l)
    c[:, o + 776:o + 904] = (s > l)
    c[:, o + 904:o + 1032] = np.where(l < s, -30000.0, 0.0)
    c[:, o + 1032:o + 1160] = np.eye(128)
    c[:, o + 1160:o + 1288] = 1.0
    c[:, o + 1288:o + 1416] = (l == (s + 64) % 128)
    c[:, o + 1416:o + 1544] = np.arange(128)[None, :]
    return c


def prep_layer1(inp):
    f = lambda a: np.asarray(a, np.float32)
    w = f(inp["w_in1"])
    qc, kc, vc, qdc, kdc, vdc, goc, glc = np.split(w, np.cumsum([1024, 256, 256, 512, 512, 1024, 1024, 16])[:-1], axis=1)
    order = []
    for j in range(2):
        for r in range(4):
            for e in range(2):
                hh = (2 * j + e) * 4 + r
                order.extend(range(hh * 64, hh * 64 + 64))
    order = np.array(order)
    w_in1p = np.concatenate([qc[:, order], kc, qdc, kdc, goc, glc, vc, vdc], axis=1)
    wo = f(inp["w_out1"])
    w_out1p = np.concatenate([wo[0:1024][order], wo[1024:]], axis=0)
    p1 = np.zeros((128, 64), np.float32)
    p1[:, 0:16] = f(inp["norm1_mix"]).reshape(16, 128).T
    p1[:, 16:32] = f(inp["norm1_ffn"]).reshape(16, 128).T
    p1[:, 32] = np.tile(f(inp["swa_q_norm"]), 2)
    p1[:, 33] = np.tile(f(inp["swa_k_norm"]), 2)
    sinks = f(inp["swa_sinks"])
    for j in range(2):
        for r in range(4):
            for e in range(2):
                p1[64 * e:64 * e + 64, 34 + j * 4 + r] = sinks[(2 * j + e) * 4 + r]
    p1[:, 42:46] = f(inp["gla_gate_b"]).reshape(4, 128).T
    p1[:, 46:48] = f(inp["gla_norm_w"]).reshape(2, 128).T
    return {
        "w_in1p": np.ascontiguousarray(w_in1p), "w_out1p": np.ascontiguousarray(w_out1p),
        "ffn1_w1": f(inp["ffn1_w1"]), "ffn1_w3": f(inp["ffn1_w3"]), "ffn1_w2": f(inp["ffn1_w2"]),
        "params1": p1, "gla_gate_w2": f(inp["gla_gate_w2"]),
    }


def prep_layer0(inp):
    f = lambda a: np.asarray(a, np.float32)
    w = f(inp["w_in0"])
    u, z, xbc, dtc = np.split(w, np.cumsum([512, 1536, 2560, 24])[:-1], axis=1)
    w_in0p = np.concatenate([u, xbc, z, dtc], axis=1)
    p = np.arange(128)
    p0 = np.zeros((128, 320), np.float32)
    p0[:, 0:16] = f(inp["norm0_mix"]).reshape(16, 128).T
    p0[:, 16:32] = f(inp["norm0_ffn"]).reshape(16, 128).T
    p0[:, 32:36] = f(inp["s5_D"]).reshape(4, 128).T
    p0[:, 36:40] = f(inp["s5_glu_b"]).reshape(4, 128).T
    p0[:, 40:60] = f(inp["ssd_conv_b"]).reshape(20, 128).T
    cw = f(inp["ssd_conv_w"])
    p0[:, 60:140] = cw.reshape(4, 20, 128).transpose(2, 1, 0).reshape(128, 80)
    p0[:, 140:164] = f(inp["ssd_A_log"])[None, :]
    p0[:, 164:188] = f(inp["ssd_D"])[None, :]
    p0[:, 188:220] = f(inp["s5_A_re"])[:, p % 64].T
    p0[:, 220:252] = f(inp["s5_A_im"])[:, p % 64].T
    p0[:, 252:284] = f(inp["s5_log_dt"])[None, :]
    p0[:, 284:308] = f(inp["ssd_dt_bias"])[None, :]
    gidx = (8 * np.arange(4)[None, :] + (p // 16)[:, None])
    ch = np.zeros((128, 5, 4, 64), np.float32)
    ch[:, 0] = f(inp["s5_A_re"])[gidx]
    ch[:, 1] = f(inp["s5_A_im"])[gidx]
    ch[:, 2] = f(inp["s5_log_dt"])[gidx][:, :, None]
    ch[:, 3] = f(inp["s5_B_re"])[gidx, :, (p % 16)[:, None]]
    ch[:, 4] = f(inp["s5_B_im"])[gidx, :, (p % 16)[:, None]]
    Cre, Cim = f(inp["s5_C_re"]), f(inp["s5_C_im"])
    s5C = np.zeros((128, 32, 2, 16), np.float32)
    s5C[0:64, :, 0, :] = Cre.transpose(2, 0, 1)
    s5C[64:128, :, 0, :] = Cim.transpose(2, 0, 1)
    s5C[0:64, :, 1, :] = Cim.transpose(2, 0, 1)
    s5C[64:128, :, 1, :] = Cre.transpose(2, 0, 1)
    return {
        "w_in0p": np.ascontiguousarray(w_in0p), "w_out0": f(inp["w_out0"]),
        "ffn0_w1": f(inp["ffn0_w1"]), "ffn0_w3": f(inp["ffn0_w3"]), "ffn0_w2": f(inp["ffn0_w2"]),
        "params0": p0, "s5ch": np.ascontiguousarray(ch.reshape(128, 5, 256)), "s5C": s5C,
        "ssd_nw": np.ascontiguousarray(np.broadcast_to(f(inp["ssd_norm_w"])[None, :], (128, 1536))),
        "s5_glu_w": f(inp["s5_glu_w"]),
    }


def pack_weights(arrs, wreg):
    wp = np.zeros((128, WTOT), np.float32)
    for pieces, k0, nk, off, n in wreg:
        blk = np.concatenate([arrs[nm][k0 * 128:(k0 + nk) * 128, c0:c0 + nc_] for (nm, c0, nc_) in pieces], axis=1)
        wp[:, off:off + n] = blk.reshape(nk, 128, -1).transpose(1, 0, 2).reshape(128, n)
    return wp


_CACHE = {}


def kernel(**inputs):
    x = np.asarray(inputs["x"], np.float32)
    Bn, T, _ = x.shape
    if "nc" not in _CACHE:
        b = Builder(T, (0, 1))
        _CACHE["nc"] = b.build()
        _CACHE["names"] = set(b.din.keys())
        _CACHE["wreg"] = b.wreg
    nc = _CACHE["nc"]
    common = {"consts": make_consts()}
    common.update(prep_layer0(inputs))
    common.update(prep_layer1(inputs))
    common["wpack"] = pack_weights(common, _CACHE["wreg"])
    common = {k: v for k, v in common.items() if k in _CACHE["names"]}
    in_maps = []
    for i in range(Bn):
        m = dict(common)
        m["xT"] = np.ascontiguousarray(x[i].T)
        in_maps.append(m)
    res = run_bass_kernel_spmd(nc, in_maps, core_ids=list(range(Bn)))
    out = np.empty((Bn, T, D), np.float32)
    for i in range(Bn):
        out[i] = np.asarray(res.results[i]["outT"]).T
    return out
```

```python
import numpy as np
import concourse.bass as bass
import concourse.mybir as mybir
from concourse.bass_utils import run_bass_kernel_spmd
from contextlib import ExitStack

F32 = mybir.dt.float32
BF16 = mybir.dt.bfloat16
I32 = mybir.dt.int32
AF = mybir.ActivationFunctionType
ALU = mybir.AluOpType

D = 2048
KT = 16
TT = 512
FH = 5632
HT = 44
NB = TT // 128
EPS = 1e-6
PI = float(np.pi)
NDMA = 8
WTOT = (2048 * 4632 + 2048 * 4624 + 2 * 2048 * 2048 + 6 * 2048 * 5632) // 128


class Op:
    __slots__ = ("eng", "fn", "deps", "isdma", "flag", "sem", "val")


class Sched:
    ENGS = ("pe", "act", "dve", "pool", "sp")

    def __init__(self):
        self.ops = {e: [] for e in self.ENGS}
        self.last_w = {}
        self.readers = {}
        self.dmas = {e: [] for e in self.ENGS}

    def add(self, eng, fn, reads=(), writes=(), dma=False):
        op = Op()
        op.eng, op.fn, op.isdma, op.flag, op.sem, op.val = eng, fn, dma, False, None, 0
        deps = {}
        for r in reads:
            w = self.last_w.get(r)
            if w is not None:
                deps[id(w)] = (w, True)
        for k in writes:
            w = self.last_w.get(k)
            if w is not None and id(w) not in deps:
                deps[id(w)] = (w, False)
            for rd in self.readers.get(k, ()):
                if id(rd) not in deps:
                    deps[id(rd)] = (rd, False)
        final = []
        for d, raw in deps.values():
            if d.eng == eng and not d.isdma and not dma:
                if (not raw) or eng == "pe":
                    continue
            final.append(d)
        if dma:
            lst = self.dmas[eng]
            if len(lst) >= NDMA:
                final.append(lst[-NDMA])
            lst.append(op)
        op.deps = final
        for r in reads:
            self.readers.setdefault(r, []).append(op)
        for k in writes:
            self.last_w[k] = op
            self.readers[k] = []
        self.ops[eng].append(op)
        return op

    PERSIST = ("wbuf", "wscr", "h", "out")
    BENGS = ("pe", "act", "dve", "pool")

    def barrier(self, full=False):
        engs = self.ENGS if full else self.BENGS
        lasts = []
        for e in engs:
            real = [o for o in self.ops[e][-3:] if o.fn is not None and not o.isdma]
            if real:
                lasts.append(real[-1])
            if full:
                lasts.extend(self.dmas[e][-NDMA:])
        for e in engs:
            op = Op()
            op.eng, op.fn, op.isdma, op.flag, op.sem, op.val = e, None, False, False, None, 0
            op.deps = [d for d in lasts if not (d.eng == e and not d.isdma)]
            self.ops[e].append(op)
        keep = lambda k: isinstance(k, tuple) and k[0] in self.PERSIST
        self.last_w = {k: v for k, v in self.last_w.items() if keep(k)}
        self.readers = {k: v for k, v in self.readers.items() if keep(k)}

    def emit(self, nc, es):
        csem = {e: es.enter_context(nc.semaphore("c_" + e)) for e in self.ENGS}
        dsem = {e: [es.enter_context(nc.semaphore("d_%s%d" % (e, i))) for i in range(NDMA)] for e in ("sp", "pool", "act")}
        for e in self.ENGS:
            for op in self.ops[e]:
                for d in op.deps:
                    d.flag = True
        for e in self.ENGS:
            cnt = 0
            k = 0
            for op in self.ops[e]:
                if op.isdma:
                    op.sem = dsem[e][k % NDMA]
                    op.val = 16 * (k // NDMA + 1)
                    k += 1
                elif op.flag:
                    if op.fn is None:
                        raise RuntimeError("dep on barrier op")
                    cnt += 1
                    op.sem = csem[e]
                    op.val = cnt
        block = es.enter_context(nc.Block())

        def run(e, eng):
            seen = {}
            for op in self.ops[e]:
                for d in op.deps:
                    if seen.get(d.sem, 0) < d.val:
                        eng.wait_ge(d.sem, d.val)
                        seen[d.sem] = d.val
                if op.fn is None:
                    continue
                ins = op.fn(eng)
                if op.isdma:
                    ins.then_inc(op.sem, 16)
                elif op.flag:
                    ins.then_inc(op.sem, 1)

        @block.tensor
        def _(eng):
            run("pe", eng)

        @block.scalar
        def _(eng):
            run("act", eng)

        @block.vector
        def _(eng):
            run("dve", eng)

        @block.gpsimd
        def _(eng):
            run("pool", eng)

        @block.sync
        def _(eng):
            run("sp", eng)


def _keys(name, idxs):
    return [(name, i) for i in idxs]


class Builder:
    def __init__(self, T, layers=(0, 1), dbg=None):
        self.T = T
        self.NT = T // TT
        self.layers = layers
        self.dbg = dbg
        self.nc = bass.Bass("TRN2", target_bir_lowering=False)
        self.S = Sched()
        self.din = {}
        self.wrr = 0
        self.wmap = {}
        self.wreg = []
        self.wtot = 0

    def sb(self, name, shape, dt):
        self.uid = getattr(self, "uid", 0) + 1
        return self.nc.sbuf_tensor("%s_%d" % (name, self.uid), shape, dt)

    def dram_in(self, name, shape):
        ap = self.nc.dram_tensor(name, list(shape), F32, kind="ExternalInput").ap()
        self.din[name] = ap
        return ap

    def mm(self, out, lhsT, rhs, start, stop, r, w):
        self.S.add("pe", lambda e: e.matmul(out, lhsT=lhsT, rhs=rhs, start=start, stop=stop), r, w)

    def tr(self, out, in_, ident, r, w):
        self.S.add("pe", lambda e: e.transpose(out, in_, ident), r, w)

    def act(self, out, in_, func, r, w, **kw):
        self.S.add("act", lambda e: e.activation(out=out, in_=in_, func=func, **kw), r, w)

    def tt(self, eng, out, in0, in1, op, r, w):
        self.S.add(eng, lambda e: e.tensor_tensor(out=out, in0=in0, in1=in1, op=op), r, w)

    def ts(self, eng, out, in0, s1, s2, op0, op1, r, w):
        if op1 is None:
            self.S.add(eng, lambda e: e.tensor_scalar(out=out, in0=in0, scalar1=s1, scalar2=None, op0=op0), r, w)
        else:
            self.S.add(eng, lambda e: e.tensor_scalar(out=out, in0=in0, scalar1=s1, scalar2=s2, op0=op0, op1=op1), r, w)

    def stt(self, eng, out, in0, scalar, in1, op0, op1, r, w):
        self.S.add(eng, lambda e: e.scalar_tensor_tensor(out=out, in0=in0, scalar=scalar, in1=in1, op0=op0, op1=op1), r, w)

    def cp(self, eng, out, in_, r, w):
        if eng == "act":
            self.S.add("act", lambda e: e.activation(out=out, in_=in_, func=AF.Copy), r, w)
        else:
            self.S.add(eng, lambda e: e.tensor_copy(out=out, in_=in_), r, w)

    def memset(self, eng, ap, val, w):
        self.S.add(eng, lambda e: e.memset(ap, val), (), w)

    def dma(self, eng, out, in_, r, w):
        self.S.add(eng, lambda e: e.dma_start(out=out, in_=in_), r, w, dma=True)

    def load(self, out, in_, w, cast=False):
        self.dma("pool" if cast else "sp", out, in_, (), w)

    def stream_w(self, wname, c0, ncols, nk=KT, k0=0, pieces=None):
        if pieces is None:
            pieces = ((wname, c0, ncols),)
        key = (tuple(pieces), k0, nk)
        ncols = sum(p[2] for p in pieces)
        n = nk * ncols
        if key not in self.wmap:
            self.wmap[key] = (len(self.wreg), self.wtot)
            self.wreg.append((tuple(pieces), k0, nk, self.wtot, n))
            off = self.wtot
            self.wtot += n
            bid = self.wmap[key][0]
            self.dma("pool", self.wscr[:, off:off + n], self.wpack[:, off:off + n], (), [("wscr", bid)])
        bid, off = self.wmap[key]
        i = self.wrr % len(self.wbuf)
        self.wrr += 1
        buf = self.wbuf[i]
        view = buf[:, 0:n].rearrange("p (k n) -> p k n", n=ncols)
        self.dma("sp", buf[:, 0:n], self.wscr[:, off:off + n], [("wscr", bid)], [("wbuf", i)])
        return view, ("wbuf", i)

    def rmsnorm(self, wcol, xn):
        h, sq, ps, rstd = self.h, self.sq, self.ps, self.rstd
        for k in range(KT):
            self.act(sq[:, k % 2, :], h[:, k, :], AF.Square, [("h", k)], [("sq", k % 2)])
            self.mm(ps[6][:, 0:TT], self.ones_bf[:], sq[:, k % 2, :], k == 0, k == KT - 1, [("sq", k % 2), "consts"], [("ps", 6)])
        self.act(rstd[:], ps[6][:, 0:TT], AF.Ln, [("ps", 6)], ["rstd"], scale=1.0 / D, bias=self.eps_col[:, 0:1])
        self.act(rstd[:], rstd[:], AF.Exp, ["rstd"], ["rstd"], scale=-0.5)
        for k in range(KT):
            self.stt("dve", xn[:, k, :], h[:, k, :], wcol[:, k:k + 1], rstd[:], ALU.mult, ALU.mult,
                     [("h", k), "rstd", "params"], [("xn", k)])

    def proj_fm(self, Wd, c0, ntiles, xn, evac, nk=KT, xkeys=None):
        if xkeys is None:
            xkeys = [("xn", k) for k in range(nk)]
        if isinstance(xn, list):
            class _L:
                def __init__(s_, l):
                    s_.l = l
                def __getitem__(s_, idx):
                    return s_.l[idx[1]]
            xn = _L(xn)
        j = 0
        if nk > KT:
            nh = nk // 2
            for j in range(ntiles):
                b = self.gb % 4
                self.gb += 1
                for hf in range(2):
                    wv, wk = self.stream_w(Wd, c0 + j * 128, 128, nh, k0=hf * nh)
                    for k in range(nh):
                        kk = hf * nh + k
                        self.mm(self.ps[b][:, 0:TT], wv[:, k, :], xn[:, kk, :], kk == 0, kk == nk - 1,
                                [wk, xkeys[kk]], [("ps", b)])
                evac(j, self.ps[b], ("ps", b))
            return
        while j < ntiles:
            nt = min(2, ntiles - j)
            wv, wk = self.stream_w(Wd, c0 + j * 128, nt * 128, nk)
            for jj in range(nt):
                b = self.gb % 4
                self.gb += 1
                for k in range(nk):
                    self.mm(self.ps[b][:, 0:TT], wv[:, k, jj * 128:(jj + 1) * 128], xn[:, k, :], k == 0, k == nk - 1,
                            [wk, xkeys[k]], [("ps", b)])
                evac(j + jj, self.ps[b], ("ps", b))
            j += nt

    def add_resid(self, Wd, rhs, nk, rkeys):
        def evac(j, ps, pk):
            self.tt("dve", self.h[:, j, :], self.h[:, j, :], ps[:, 0:TT], ALU.add, [("h", j), pk], [("h", j)])
        self.proj_fm(Wd, 0, KT, rhs, evac, nk=nk, xkeys=rkeys)

    def ffn(self, wcol, W1, W3, W2, es):
        xn = self.xn
        self.rmsnorm(wcol, xn)
        g = es.enter_context(self.sb("ffn_g", [128, HT, TT], BF16))
        tmp = es.enter_context(self.sb("ffn_tmp", [128, 2, TT], F32))
        for i in range(HT):
            wv, wk = self.stream_w(None, 0, 0, pieces=((W1, i * 128, 128), (W3, i * 128, 128)))
            for k in range(KT):
                self.mm(self.ps[0][:, 0:TT], wv[:, k, 0:128], xn[:, k, :], k == 0, k == KT - 1, [wk, ("xn", k)], [("ps", 0)])
            for k in range(KT):
                self.mm(self.ps[1][:, 0:TT], wv[:, k, 128:256], xn[:, k, :], k == 0, k == KT - 1, [wk, ("xn", k)], [("ps", 1)])
            self.act(tmp[:, i % 2, :], self.ps[0][:, 0:TT], AF.Silu, [("ps", 0)], [("ftmp", i % 2)])
            self.tt("dve", g[:, i, :], tmp[:, i % 2, :], self.ps[1][:, 0:TT], ALU.mult, [("ftmp", i % 2), ("ps", 1)], [("g", i)])
        self.add_resid(W2, g, HT, [("g", i) for i in range(HT)])

    def layer1_setup(self, es):
        nc = self.nc
        E = es.enter_context
        d = self.dram_in
        self.w_in1 = "w_in1p"
        self.w_out1 = "w_out1p"
        self.w1_1, self.w3_1, self.w2_1 = "ffn1_w1", "ffn1_w3", "ffn1_w2"
        p1 = d("params1", [128, 64])
        gw2 = d("gla_gate_w2", [16, 512])
        self.p1 = E(self.sb("p1", [128, 64], F32))
        self.load(self.p1[:], p1[:, :], ["params"])
        self.gw2_bf = E(self.sb("gw2_bf", [16, 512], BF16))
        self.load(self.gw2_bf[:], gw2[:, :], ["params"], cast=True)
        self.sinkE = E(self.sb("sinkE", [128, 8], F32))
        self.negb = E(self.sb("negb", [128, 4], F32))
        self.act(self.sinkE[:], self.p1[:, 34:42], AF.Exp, ["params"], ["params2"])
        self.ts("dve", self.negb[:], self.p1[:, 42:46], -1.0, None, ALU.mult, None, ["params"], ["params2"])
        self.kn_hist = E(self.sb("kn_hist", [128, 2, 128], BF16))
        self.Vpad = E(self.sb("Vpad", [128, 5, 4, 128], BF16))
        self.memset("dve", self.Vpad[:], 0.0, ["Vpad"])
        self.memset("dve", self.kn_hist[:], 0.0, ["knh"])
        self.Sf = E(self.sb("gla_Sf", [128, 4, 256], F32))
        self.Sb = E(self.sb("gla_Sb", [128, 4, 256], BF16))
        self.memset("dve", self.Sf[:], 0.0, [("Sf", i) for i in range(4)])
        self.memset("dve", self.Sb[:], 0.0, [("Sb", i) for i in range(4)])

    def layer1(self, ti):
        nc, ps = self.nc, self.ps
        p1 = self.p1
        with ExitStack() as es:
            E = es.enter_context
            xn = self.xn
            self.rmsnorm(p1[:, 0:16], xn)
            ymix = E(self.sb("ymix1", [128, 16, TT], BF16))
            qd = E(self.sb("qd", [128, 4, TT], BF16))
            kd = E(self.sb("kd", [128, 4, TT], BF16))
            sg = E(self.sb("sg", [128, 8, TT], BF16))
            glr = E(self.sb("glr", [16, TT], BF16))
            vd = E(self.sb("vd_tok", [128, NB, 1024], BF16))
            esA = ExitStack()
            EA = esA.enter_context
            qn = EA(self.sb("qn", [128, 10, 128 + TT], BF16))
            qkf = EA(self.sb("qkf", [128, 2, TT], F32))
            sq2 = EA(self.sb("sq2", [128, 2, TT], BF16))
            rs2 = EA(self.sb("rs2", [128, 2, TT], F32))
            self.cp("pool", qn[:, 8:10, 0:128], self.kn_hist[:], ["knh"], [("qn", 8), ("qn", 9)])

            def evac_qk(j, pst, pk):
                i = j % 2
                self.cp("act", qkf[:, i, :], pst[:, 0:TT], [pk], [("qkf", i)])
                self.act(sq2[:, i, :], pst[:, 0:TT], AF.Square, [pk], [("sq2", i)])
                self.mm(ps[6][:, 0:TT], self.bones_bf[:], sq2[:, i, :], True, True, [("sq2", i), "consts"], [("ps", 6)])
                self.act(rs2[:, i, :], ps[6][:, 0:TT], AF.Ln, [("ps", 6)], [("rs2", i)], scale=1.0 / 64, bias=self.eps_col[:, 0:1])
                self.act(rs2[:, i, :], rs2[:, i, :], AF.Exp, [("rs2", i)], [("rs2", i)], scale=-0.5)
                wc = p1[:, 32:33] if j < 8 else p1[:, 33:34]
                self.stt("dve", qn[:, j, 128:128 + TT], qkf[:, i, :], wc, rs2[:, i, :], ALU.mult, ALU.mult,
                         [("qkf", i), ("rs2", i), "params"], [("qn", j)])
            self.proj_fm(self.w_in1, 0, 10, xn, evac_qk)

            def evac_qd(j, pst, pk):
                self.act(qd[:, j, :], pst[:, 0:TT], AF.Copy, [pk], [("qd", j)], scale=128.0 ** -0.5)
            self.proj_fm(self.w_in1, 1280, 4, xn, evac_qd)

            def evac_kd(j, pst, pk):
                self.cp("act", kd[:, j, :], pst[:, 0:TT], [pk], [("kd", j)])
            self.proj_fm(self.w_in1, 1792, 4, xn, evac_kd)

            def evac_sg(j, pst, pk):
                self.act(sg[:, j, :], pst[:, 0:TT], AF.Silu, [pk], [("sg", j)])
            self.proj_fm(self.w_in1, 2304, 8, xn, evac_sg)
            wv, wk = self.stream_w(self.w_in1, 3328, 16)
            for k in range(KT):
                self.mm(ps[0][0:16, 0:TT], wv[:, k, :], xn[:, k, :], k == 0, k == KT - 1, [wk, ("xn", k)], [("ps", 0)])
            self.cp("act", glr[:], ps[0][0:16, 0:TT], [("ps", 0)], ["glr"])
            wv, wk = self.stream_w(self.w_in1, 3344, 256)
            for b in range(NB):
                for k in range(KT):
                    self.mm(ps[1][:, 0:256], xn[:, k, b * 128:(b + 1) * 128], wv[:, k, :], k == 0, k == KT - 1, [wk, ("xn", k)], [("ps", 1)])
                src = ps[1][:, 0:256].rearrange("p (h d) -> p h d", d=64)
                for e in range(2):
                    self.cp("act", self.Vpad[:, b + 1, e::2, 64 * e:64 * e + 64], src[:, e::2, :], [("ps", 1)], [("Vpad", b + 1)])
            for qr in range(4):
                wv, wk = self.stream_w(self.w_in1, 3600 + qr * 256, 256)
                for b in range(NB):
                    pb = self.gb % 4
                    self.gb += 1
                    for k in range(KT):
                        self.mm(ps[pb][:, 0:256], xn[:, k, b * 128:(b + 1) * 128], wv[:, k, :], k == 0, k == KT - 1, [wk, ("xn", k)], [("ps", pb)])
                    self.cp("act", vd[:, b, qr * 256:(qr + 1) * 256], ps[pb][:, 0:256], [("ps", pb)], [("vd", b, qr)])

            pT = EA(self.sb("pT", [128, 4, 512], BF16))
            den = EA(self.sb("den", [128, 512], F32))
            for j in range(2):
                for b in range(NB):
                    first = (ti == 0 and b == 0)
                    kbs = [1] if first else [0, 1]
                    combos = [(e, kb) for e in range(2) for kb in kbs]
                    for (e, kb) in combos:
                        sb = 2 + (e * 2 + kb) % 2
                        pi = e * 2 + kb
                        kc0 = b * 128 + kb * 128
                        self.mm(ps[sb][:, 0:512].rearrange("p (r q) -> p r q", q=128),
                                qn[64 * e:64 * e + 64, 8 + j, kc0:kc0 + 128],
                                qn[64 * e:64 * e + 64, 4 * j:4 * j + 4, 128 + b * 128:128 + (b + 1) * 128],
                                True, False, [("qn", 8 + j)] + [("qn", 4 * j + r) for r in range(4)], [("ps", sb)])
                        self.mm(ps[sb][:, 0:512], self.ident_bf[:], self.maskPC[:, kb, :], False, True, ["consts"], [("ps", sb)])
                        self.act(pT[:, pi, :], ps[sb][:, 0:512], AF.Exp, [("ps", sb)], [("pT", pi)], scale=0.125)
                    n = len(combos)
                    for i, (e, kb) in enumerate(combos):
                        pi = e * 2 + kb
                        self.mm(ps[4][:, 0:512], self.Vpad[:, b + kb, 2 * j + e, :], pT[:, pi, :], i == 0, i == n - 1,
                                [("Vpad", b + kb), ("pT", pi)], [("ps", 4)])
                    for i, (e, kb) in enumerate(combos):
                        pi = e * 2 + kb
                        self.mm(ps[5][:, 0:512], self.onespad[:, e, :], pT[:, pi, :], i == 0, i == n - 1,
                                ["consts", ("pT", pi)], [("ps", 5)])
                    self.tt("dve", den[:].rearrange("p (r q) -> p r q", q=128), ps[5][:, 0:512].rearrange("p (r q) -> p r q", q=128),
                            self.sinkE[:, 4 * j:4 * j + 4].unsqueeze(2).broadcast_to([128, 4, 128]), ALU.add,
                            [("ps", 5), "params2"], ["den"])
                    self.S.add("dve", lambda e_, den=den: e_.reciprocal(out=den[:], in_=den[:]), ["den"], ["den"])
                    self.tt("dve", ymix[:, 4 * j:4 * j + 4, b * 128:(b + 1) * 128],
                            ps[4][:, 0:512].rearrange("p (r q) -> p r q", q=128),
                            den[:].rearrange("p (r q) -> p r q", q=128), ALU.mult,
                            [("ps", 4), "den"], [("ymix", 4 * j + r) for r in range(4)])
            self.cp("pool", self.kn_hist[:], qn[:, 8:10, TT:TT + 128], [("qn", 8), ("qn", 9)], ["knh"])
            self.cp("pool", self.Vpad[:, 0, :, :], self.Vpad[:, NB, :, :], [("Vpad", NB)], [("Vpad", 0)])

            self.S.barrier()
            esA.close()
            ef = E(self.sb("gla_e", [128, TT], F32))
            bcp = E(self.sb("gla_bc", [128, TT], F32))
            Ep = E(self.sb("gla_Ep", [128, TT], F32))
            En = E(self.sb("gla_En", [128, TT], F32))
            qt = E(self.sb("gla_qt", [128, TT], BF16))
            kt_ = E(self.sb("gla_kt", [128, TT], BF16))
            ktok = E(self.sb("gla_ktok", [128, NB, 128], BF16))
            nbl = E(self.sb("gla_nb", [128, 8], F32))
            dec = E(self.sb("gla_dec", [128, 8], F32))
            attm = E(self.sb("gla_attm", [128, 2, 128], BF16))
            of = E(self.sb("gla_of", [128, 2, TT], F32))
            osq = E(self.sb("gla_osq", [128, 2, TT], BF16))
            for hd in range(4):
                self.mm(ps[0][:, 0:TT], self.gw2_bf[0:16, hd * 128:(hd + 1) * 128], glr[0:16, :], True, True, ["params", "glr"], [("ps", 0)])
                self.act(ef[:], ps[0][:, 0:TT], AF.Exp, [("ps", 0), "params2"], ["ef"], scale=-1.0, bias=self.negb[:, hd:hd + 1])
                self.act(ef[:], ef[:], AF.Ln, ["ef"], ["ef"], bias=self.one_col[:, 0:1])
                self.S.add("dve", lambda e_, bcp=bcp, ef=ef: e_.tensor_tensor_scan(out=bcp[:], data0=self.segm[:], data1=ef[:], initial=0.0, op0=ALU.mult, op1=ALU.add),
                           ["ef", "consts"], ["bcp"])
                self.act(Ep[:], bcp[:], AF.Exp, ["bcp"], ["Ep"], scale=-1.0 / 16)
                self.act(En[:], bcp[:], AF.Exp, ["bcp"], ["En"], scale=1.0 / 16)
                self.tt("dve", qt[:], qd[:, hd, :], Ep[:], ALU.mult, [("qd", hd), "Ep"], ["qt"])
                self.tt("dve", kt_[:], kd[:, hd, :], En[:], ALU.mult, [("kd", hd), "En"], ["kt"])
                self.act(dec[:], bcp[:, 63::64], AF.Exp, ["bcp"], ["dec"], scale=-1.0 / 16)
                for b in range(NB):
                    self.tr(self.psT[:, 0:128], kt_[:, b * 128:(b + 1) * 128], self.ident_bf[:], ["kt", "consts"], [("ps", 7)])
                    self.cp("act", ktok[:, b, :], self.psT[:, 0:128], [("ps", 7)], [("ktok", b)])
                for b in range(NB):
                    bs = slice(b * 128, (b + 1) * 128)
                    self.mm(ps[4][:, 0:128], kt_[:, bs], qt[:, bs], True, True, ["kt", "qt"], [("ps", 4)])
                    self.tt("dve", attm[:, b % 2, :], ps[4][:, 0:128], self.gmask[:], ALU.mult, [("ps", 4), "consts"], [("attm", b % 2)])
                    for c in range(2):
                        ci = b * 2 + c
                        cs = slice(b * 128 + c * 64, b * 128 + (c + 1) * 64)
                        for vt in range(2):
                            vcol = slice(hd * 256 + vt * 128, hd * 256 + (vt + 1) * 128)
                            self.mm(ps[2 + vt][:, cs], vd[:, b, vcol], attm[:, b % 2, c * 64:(c + 1) * 64], True, False,
                                    [("vd", b, hd), ("attm", b % 2)], [("ps", 2 + vt)])
                            self.mm(ps[2 + vt][:, cs], self.Sb[:, hd, vt * 128:(vt + 1) * 128], qt[:, cs], False, True,
                                    [("Sb", hd), "qt"], [("ps", 2 + vt)])
                        self.mm(ps[5][:, 0:256], ktok[64 * c:64 * c + 64, b, :], vd[64 * c:64 * c + 64, b, hd * 256:(hd + 1) * 256], True, True,
                                [("ktok", b), ("vd", b, hd)], [("ps", 5)])
                        self.tt("dve", self.Sf[:, hd, :], self.Sf[:, hd, :], ps[5][:, 0:256], ALU.add, [("Sf", hd), ("ps", 5)], [("Sf", hd)])
                        self.act(self.Sb[:, hd, :], self.Sf[:, hd, :], AF.Copy, [("Sf", hd), "dec"], [("Sb", hd)], scale=dec[:, ci:ci + 1])
                        self.act(self.Sf[:, hd, :], self.Sf[:, hd, :], AF.Copy, [("Sf", hd), "dec"], [("Sf", hd)], scale=dec[:, ci:ci + 1])
                for vt in range(2):
                    self.cp("act", of[:, vt, :], ps[2 + vt][:, 0:TT], [("ps", 2 + vt)], [("of", vt)])
                    self.act(osq[:, vt, :], ps[2 + vt][:, 0:TT], AF.Square, [("ps", 2 + vt)], [("osq", vt)])
                for vt in range(2):
                    self.mm(ps[6][:, 0:TT], self.ones_bf[:], osq[:, vt, :], vt == 0, vt == 1, [("osq", vt), "consts"], [("ps", 6)])
                self.act(ef[:], ps[6][:, 0:TT], AF.Ln, [("ps", 6)], ["ef"], scale=1.0 / 256, bias=self.eps_col[:, 0:1])
                self.act(ef[:], ef[:], AF.Exp, ["ef"], ["ef"], scale=-0.5)
                for vt in range(2):
                    self.stt("dve", of[:, vt, :], of[:, vt, :], p1[:, 46 + vt:47 + vt], ef[:], ALU.mult, ALU.mult,
                             [("of", vt), "ef", "params"], [("of", vt)])
                    self.tt("dve", ymix[:, 8 + hd * 2 + vt, :], of[:, vt, :], sg[:, hd * 2 + vt, :], ALU.mult,
                            [("of", vt), ("sg", hd * 2 + vt)], [("ymix", 8 + hd * 2 + vt)])
            self.add_resid(self.w_out1, ymix, KT, [("ymix", i) for i in range(KT)])
        self.S.barrier()
        with ExitStack() as es:
            self.ffn(p1[:, 16:32], self.w1_1, self.w3_1, self.w2_1, es)
        self.S.barrier()

    def sin_of(self, out, x, shift, tmp, tmpi, key):
        c = (shift + 5 * PI) / (2 * PI) - 0.5
        self.ts("dve", tmp, x, 1.0 / (2 * PI), c, ALU.mult, ALU.add, [key], [key + "_t"])
        self.cp("dve", tmpi, tmp, [key + "_t"], [key + "_i"])
        self.cp("dve", tmp, tmpi, [key + "_i"], [key + "_t"])
        self.stt("dve", tmp, tmp, -2 * PI, x, ALU.mult, ALU.add, [key + "_t", key], [key + "_t"])
        self.ts("dve", tmp, tmp, shift + 4 * PI, None, ALU.add, None, [key + "_t"], [key + "_t"])
        self.ts("dve", tmp, tmp, -PI, PI, ALU.max, ALU.min, [key + "_t"], [key + "_t"])
        self.act(out, tmp, AF.Sin, [key + "_t"], [key + "_o"])

    def layer0_setup(self, es):
        nc = self.nc
        E = es.enter_context
        d = self.dram_in
        cf = self.cf
        self.sgn = cf[:, 642:643]
        self.m4 = cf[:, 643:647]
        self.TriF = cf[:, 648:776]
        self.UstrF = cf[:, 776:904]
        self.negmF = cf[:, 904:1032]
        self.ident_f = cf[:, 1032:1160]
        self.ones_f = cf[:, 1160:1288]
        self.swap_f = cf[:, 1288:1416]
        self.tpos = cf[:, 1416:1544]
        self.w_in0 = "w_in0p"
        self.w_out0 = "w_out0"
        self.w1_0, self.w3_0, self.w2_0 = "ffn0_w1", "ffn0_w3", "ffn0_w2"
        p0d = d("params0", [128, 320])
        s5ch_d = d("s5ch", [128, 5, 256])
        s5C_d = d("s5C", [128, 32, 2, 16])
        nw_d = d("ssd_nw", [128, 1536])
        glu_d = d("s5_glu_w", [512, 512])
        p0 = self.p0 = E(self.sb("p0", [128, 320], F32))
        self.load(p0[:], p0d[:, :], ["params"])
        self.Cc = E(self.sb("s5Cc", [128, 32, 2, 16], BF16))
        self.load(self.Cc[:], s5C_d[:, :, :, :], ["params"], cast=True)
        self.nw = E(self.sb("ssdnw", [128, 1536], BF16))
        self.load(self.nw[:], nw_d[:, :], ["params"], cast=True)
        self.gluw = E(self.sb("gluw", [128, 4, 512], BF16))
        self.load(self.gluw[:], glu_d.rearrange("(k p) n -> p k n", p=128), ["params"], cast=True)
        self.COS = E(self.sb("COS", [128, 32, 128], BF16))
        self.SIN = E(self.sb("SIN", [128, 32, 128], BF16))
        self.BAc = E(self.sb("BAc", [128, 4, 4, 128], BF16))
        self.BBc = E(self.sb("BBc", [128, 4, 4, 128], BF16))
        self.diagD = E(self.sb("diagD", [128, 4, 128], BF16))
        self.rho = E(self.sb("rho", [128, 32], F32))
        self.CS2 = E(self.sb("CS2", [128, 32, 2], F32))
        self.carry = E(self.sb("carry", [128, 32], F32))
        self.STf = E(self.sb("STf", [128, 24, 64], F32))
        self.STb = E(self.sb("STb", [128, 24, 64], BF16))
        self.chist = E(self.sb("chist", [128, 20, 3], BF16))
        self.Arow = E(self.sb("Arow", [128, 24], F32))
        self.zeros_bf = E(self.sb("zeros_bf", [128, 512], BF16))
        self.memset("dve", self.zeros_bf[:], 0.0, ["zeros"])
        self.memset("dve", self.carry[:], 0.0, ["carry"])
        self.memset("dve", self.STf[:], 0.0, ["STf"])
        self.memset("dve", self.STb[:], 0.0, ["STb"])
        self.memset("dve", self.chist[:], 0.0, ["chist"])
        self.act(self.Arow[:], p0[:, 140:164], AF.Exp, ["params"], ["Arow"])
        self.ts("dve", self.Arow[:], self.Arow[:], -1.0, None, ALU.mult, None, ["Arow"], ["Arow"])
        for j in range(4):
            self.ts("dve", self.diagD[:, j, :], self.ident_f, p0[:, 32 + j:33 + j], None, ALU.mult, None, ["params", "consts"], ["diagD"])
        with ExitStack() as tes:
            TE = tes.enter_context
            ang = TE(self.sb("ang", [128, 32, 128], F32))
            tmp = TE(self.sb("sc_t", [128, 4096], F32))
            tmpi = TE(self.sb("sc_i", [128, 4096], I32))
            sm = TE(self.sb("s5sm", [128, 6, 32], F32))
            dtst, th, lam, angq, cq, sq_ = (sm[:, i, :] for i in range(6))
            self.act(dtst, p0[:, 252:284], AF.Exp, ["params"], ["dtst"])
            self.tt("dve", th, p0[:, 220:252], dtst, ALU.mult, ["params", "dtst"], ["th"])
            self.tt("dve", lam, p0[:, 188:220], dtst, ALU.mult, ["params", "dtst"], ["lam"])
            self.act(self.rho[:], lam, AF.Exp, ["lam"], ["rho"])
            self.tt("dve", ang[:], th.unsqueeze(2).broadcast_to([128, 32, 128]), self.tpos.unsqueeze(1).broadcast_to([128, 32, 128]), ALU.mult,
                    ["th", "consts"], ["ang"])
            angf = ang[:].rearrange("p g t -> p (g t)")
            self.sin_of(self.COS[:].rearrange("p g t -> p (g t)"), angf, PI / 2, tmp[:], tmpi[:], "ang")
            self.sin_of(self.SIN[:].rearrange("p g t -> p (g t)"), angf, 0.0, tmp[:], tmpi[:], "ang")
            self.ts("dve", angq, th, 128.0, None, ALU.mult, None, ["th"], ["angq"])
            self.sin_of(cq, angq, PI / 2, tmp[:, 0:32], tmpi[:, 0:32], "angq")
            self.cp("dve", self.CS2[:, :, 0], cq, ["angq_o"], ["CS2a"])
            self.sin_of(sq_, angq, 0.0, tmp[:, 0:32], tmpi[:, 0:32], "angq")
            self.ts("dve", self.CS2[:, :, 1], sq_, self.sgn, None, ALU.mult, None, ["angq_o", "consts"], ["CS2b"])
            self.S.barrier(full=True)
        with ExitStack() as tes:
            TE = tes.enter_context
            tmp = TE(self.sb("sc_t2", [128, 256], F32))
            tmpi = TE(self.sb("sc_i2", [128, 256], I32))
            ch = TE(self.sb("s5ch", [128, 5, 256], F32))
            self.load(ch[:], s5ch_d[:, :, :], ["ch"])
            w = TE(self.sb("s5w", [128, 14, 256], F32))
            Are, Aim, ldt, BreT, BimT = (ch[:, i, :] for i in range(5))
            dt, lr, thc, mag, cs, sn, abr, abi, er, den, cr, ci, u1, u2 = (w[:, i, :] for i in range(14))
            K = "chw"
            self.act(dt, ldt, AF.Exp, ["ch"], [K])
            self.tt("dve", lr, Are, dt, ALU.mult, ["ch", K], [K])
            self.tt("dve", thc, Aim, dt, ALU.mult, ["ch", K], ["thc"])
            self.act(mag, lr, AF.Exp, [K], [K])
            self.sin_of(cs, thc, PI / 2, tmp[:, 0:256], tmpi[:, 0:256], "thc")
            self.tt("dve", abr, mag, cs, ALU.mult, [K, "thc_o"], [K])
            self.sin_of(sn, thc, 0.0, tmp[:, 0:256], tmpi[:, 0:256], "thc")
            self.tt("dve", abi, mag, sn, ALU.mult, [K, "thc_o"], [K])
            self.ts("dve", er, abr, -1.0, None, ALU.add, None, [K], [K])
            self.tt("dve", den, Are, Are, ALU.mult, ["ch", K], [K])
            self.tt("dve", u1, Aim, Aim, ALU.mult, ["ch", K], [K])
            self.tt("dve", den, den, u1, ALU.add, [K], [K])
            self.S.add("dve", lambda e_: e_.reciprocal(out=den, in_=den), [K], [K])
            self.tt("dve", u1, er, Are, ALU.mult, ["ch", K], [K])
            self.tt("dve", u2, abi, Aim, ALU.mult, ["ch", K], [K])
            self.tt("dve", u1, u1, u2, ALU.add, [K], [K])
            self.tt("dve", cr, u1, den, ALU.mult, [K], [K])
            self.tt("dve", u1, abi, Are, ALU.mult, ["ch", K], [K])
            self.tt("dve", u2, er, Aim, ALU.mult, ["ch", K], [K])
            self.tt("dve", u1, u1, u2, ALU.subtract, [K], [K])
            self.tt("dve", ci, u1, den, ALU.mult, [K], [K])
            bf = TE(self.sb("s5bf", [128, 2, 4, 128], F32))
            v4 = lambda ap: ap.rearrange("p (j n) -> p j n", n=64)
            self.tt("dve", u1, BreT, cr, ALU.mult, ["ch", K], [K])
            self.tt("dve", u2, BimT, ci, ALU.mult, ["ch", K], [K])
            self.tt("dve", bf[:, 0, :, 0:64], v4(u1), v4(u2), ALU.subtract, [K], ["bf"])
            self.cp("dve", bf[:, 1, :, 64:128], bf[:, 0, :, 0:64], ["bf"], ["bf"])
            self.tt("dve", u1, BreT, ci, ALU.mult, ["ch", K], [K])
            self.tt("dve", u2, BimT, cr, ALU.mult, ["ch", K], [K])
            self.tt("dve", bf[:, 0, :, 64:128], v4(u1), v4(u2), ALU.add, [K], ["bf"])
            self.cp("dve", bf[:, 1, :, 0:64], bf[:, 0, :, 64:128], ["bf"], ["bf"])
            for q in range(4):
                self.ts("dve", self.BAc[:, :, q, :], bf[:, 0, :, :], self.m4[:, q:q + 1], None, ALU.mult, None, ["bf", "consts"], ["BAc"])
                self.ts("dve", self.BBc[:, :, q, :], bf[:, 1, :, :], self.m4[:, q:q + 1], None, ALU.mult, None, ["bf", "consts"], ["BAc"])
            self.S.barrier(full=True)

    def layer0(self, ti):
        nc, ps = self.nc, self.ps
        p0 = self.p0
        xn = self.xn
        with ExitStack() as es:
            E = es.enter_context
            self.rmsnorm(p0[:, 0:16], xn)
            ymix = E(self.sb("ymix0a", [128, 4, TT], BF16))
            esA = ExitStack()
            EA = esA.enter_context
            u_bf = EA(self.sb("u_bf", [128, 4, TT], BF16))
            t1 = EA(self.sb("s5t1", [128, 2, TT], F32))
            t2 = EA(self.sb("s5t2", [128, 2, TT], F32))
            xp = EA(self.sb("s5xp", [128, 4, TT], F32))
            gs = EA(self.sb("s5gs", [128, 4, TT], F32))
            P12 = EA(self.sb("s5P", [128, 2, 2, TT], BF16))
            v2 = EA(self.sb("s5v2", [128, 4, 2], F32))
            ygT = EA(self.sb("ygT", [128, 4, TT], BF16))

            def evac_u(j, pst, pk):
                self.cp("act", u_bf[:, j, :], pst[:, 0:TT], [pk], [("u", j)])
            self.proj_fm(self.w_in0, 0, 4, xn, evac_u)
            for b in range(NB):
                self.mm(ps[b][:, 0:512], self.ident_bf, self.zeros_bf[:], True, False, ["consts", "zeros"], [("ps", b)])
                for j in range(4):
                    self.mm(ps[b][:, j * 128:(j + 1) * 128], u_bf[:, j, b * 128:(b + 1) * 128], self.diagD[:, j, :], False, False,
                            [("u", j), "diagD"], [("ps", b)])
            v3 = lambda ap: ap.rearrange("p (c t) -> p c t", t=128)
            CB = (4, 5, 6)
            for gset in range(8):
                for i in range(4):
                    g = gset * 4 + i
                    j, gp = g // 8, g % 8
                    hf, q = gp // 4, gp % 4
                    i2 = i % 2
                    rows = slice(64 * hf, 64 * hf + 64)
                    pa, pb = 4, 5
                    self.mm(ps[pa][:, 0:TT], self.BAc[rows, j, q, :], u_bf[rows, j, :], True, True, [("u", j), "BAc"], [("ps", pa)])
                    self.mm(ps[pb][:, 0:TT], self.BBc[rows, j, q, :], u_bf[rows, j, :], True, True, [("u", j), "BAc"], [("ps", pb)])
                    cosb = self.COS[:, g, :].unsqueeze(1).broadcast_to([128, NB, 128])
                    sinb = self.SIN[:, g, :].unsqueeze(1).broadcast_to([128, NB, 128])
                    self.tt("dve", v3(t1[:, i2, :]), v3(ps[pa][:, 0:TT]), cosb, ALU.mult, [("ps", pa), "trig"], [("t1", i2)])
                    self.stt("dve", v3(t2[:, i2, :]), v3(ps[pb][:, 0:TT]), self.sgn, sinb, ALU.mult, ALU.mult, [("ps", pb), "trig", "consts"], [("t2", i2)])
                    self.tt("dve", xp[:, i, :], t1[:, i2, :], t2[:, i2, :], ALU.add, [("t1", i2), ("t2", i2)], [("xp", i)])
                for c in range(NB):
                    cs_ = slice(c * 128, (c + 1) * 128)
                    last = c * 128 + 127
                    for i in range(4):
                        g = gset * 4 + i
                        cb_ = CB[i % 3]
                        self.S.add("dve", lambda e_, g=g, i=i, cs_=cs_: e_.tensor_tensor_scan(
                            out=gs[:, i, cs_], data0=self.rho[:, g:g + 1].broadcast_to([128, 128]), data1=xp[:, i, cs_],
                            initial=self.carry[:, g:g + 1], op0=ALU.mult, op1=ALU.add),
                            [("xp", i), ("carry", g), "rho"], [("gs", i)])
                        self.tt("dve", v2[:, i, :], gs[:, i, last:last + 1].broadcast_to([128, 2]), self.CS2[:, g, :], ALU.mult,
                                [("gs", i), "CS2"], [("v2", i)])
                        self.mm(ps[cb_][:, g:g + 1], self.ident_f, v2[:, i, 0:1], True, False, [("v2", i), "consts"], [("ps", cb_)])
                        self.mm(ps[cb_][:, g:g + 1], self.swap_f, v2[:, i, 1:2], False, True, [("v2", i), "consts"], [("ps", cb_)])
                        self.cp("act", self.carry[:, g:g + 1], ps[cb_][:, g:g + 1], [("ps", cb_)], [("carry", g)])
                for i in range(4):
                    g = gset * 4 + i
                    i2 = i % 2
                    cosb = self.COS[:, g, :].unsqueeze(1).broadcast_to([128, NB, 128])
                    sinb = self.SIN[:, g, :].unsqueeze(1).broadcast_to([128, NB, 128])
                    self.stt("dve", v3(P12[:, i2, 0, :]), v3(gs[:, i, :]), self.sgn, cosb, ALU.mult, ALU.mult, [("gs", i), "trig", "consts"], [("P", i2)])
                    self.stt("dve", v3(P12[:, i2, 1, :]), v3(gs[:, i, :]), -1.0, sinb, ALU.mult, ALU.mult, [("gs", i), "trig"], [("P", i2)])
                    for b in range(NB):
                        bs = slice(b * 128, (b + 1) * 128)
                        self.mm(ps[b][:, 16 * g:16 * g + 16], P12[:, i2, 0, bs], self.Cc[:, g, 0, :], False, False, [("P", i2), "params"], [("ps", b)])
                        self.mm(ps[b][:, 16 * g:16 * g + 16], P12[:, i2, 1, bs], self.Cc[:, g, 1, :], False, True, [("P", i2), "params"], [("ps", b)])
            yf = EA(self.sb("s5yf", [128, 2, 512], F32))
            yt = EA(self.sb("s5yt", [128, 2, 512], F32))
            ygt = EA(self.sb("s5ygt", [128, 2, 512], BF16))
            GC = float(2.0 * np.sqrt(2.0 / np.pi))
            for b in range(NB):
                i2 = b % 2
                self.cp("act", yf[:, i2, :], ps[b][:, 0:512], [("ps", b)], [("yf", i2)])
                self.tt("pool", yt[:, i2, :], yf[:, i2, :], yf[:, i2, :], ALU.mult, [("yf", i2)], [("yt", i2)])
                self.ts("dve", yt[:, i2, :], yt[:, i2, :], 0.044715, 1.0, ALU.mult, ALU.add, [("yt", i2)], [("yt", i2)])
                self.tt("pool", yt[:, i2, :], yt[:, i2, :], yf[:, i2, :], ALU.mult, [("yt", i2), ("yf", i2)], [("yt", i2)])
                self.act(yt[:, i2, :], yt[:, i2, :], AF.Sigmoid, [("yt", i2)], [("yt", i2)], scale=GC)
                self.tt("dve", ygt[:, i2, :], yf[:, i2, :], yt[:, i2, :], ALU.mult, [("yf", i2), ("yt", i2)], [("ygt", i2)])
                for k in range(4):
                    self.tr(self.psT[:, k * 128:(k + 1) * 128], ygt[:, i2, k * 128:(k + 1) * 128], self.ident_bf, [("ygt", i2), "consts"], [("ps", 7)])
                self.cp("act", ygT[:, :, b * 128:(b + 1) * 128], self.psT[:, 0:512].rearrange("p (k t) -> p k t", t=128), [("ps", 7)], ["ygT"])
            for jo in range(4):
                pb = 4 + jo % 2
                for k in range(4):
                    self.mm(ps[pb][:, 0:TT], self.gluw[:, k, jo * 128:(jo + 1) * 128], ygT[:, k, :], k == 0, k == 3, ["params", "ygT"], [("ps", pb)])
                self.act(yf[:, jo % 2, :], ps[pb][:, 0:TT], AF.Sigmoid, [("ps", pb), "params"], [("yf", jo % 2)], bias=p0[:, 36 + jo:37 + jo])
                self.tt("dve", ymix[:, jo, :], ygT[:, jo, :], yf[:, jo % 2, :], ALU.mult, ["ygT", ("yf", jo % 2)], [("ymix", jo)])
            self.S.barrier()
            esA.close()
            BC = E(self.sb("BC_fm", [128, 8, TT], BF16))
            xs_tok = E(self.sb("xs_tok", [128, NB, 1536], BF16))
            B_tok = E(self.sb("B_tok", [128, NB, 512], BF16))
            esB = ExitStack()
            EB = esB.enter_context
            xpre = EB(self.sb("xbc_pre", [128, 20, 3 + TT], BF16))
            acc = EB(self.sb("cacc", [128, 2, TT], F32))
            xsf = EB(self.sb("xs_fm", [128, 2, TT], BF16))
            self.cp("pool", xpre[:, :, 0:3], self.chist[:], ["chist"], [("xpre", ct) for ct in range(20)])

            def evac_x(j, pst, pk):
                self.cp("act", xpre[:, j, 3:3 + TT], pst[:, 0:TT], [pk], [("xpre", j)])
            self.proj_fm(self.w_in0, 512, 20, xn, evac_x)
            for ct in range(20):
                i2 = ct % 2
                eng = "dve"
                cw = lambda k: p0[:, 60 + ct * 4 + k:61 + ct * 4 + k]
                self.ts(eng, acc[:, i2, :], xpre[:, ct, 3:3 + TT], cw(3), None, ALU.mult, None, [("xpre", ct), "params"], [("acc", i2)])
                for k in range(3):
                    self.stt(eng, acc[:, i2, :], xpre[:, ct, k:k + TT], cw(k), acc[:, i2, :], ALU.mult, ALU.add, [("xpre", ct), ("acc", i2), "params"], [("acc", i2)])
                if ct < 12:
                    self.act(xsf[:, i2, :], acc[:, i2, :], AF.Silu, [("acc", i2), "params"], [("xsf", i2)], bias=p0[:, 40 + ct:41 + ct])
                    src = xsf[:, i2, :]
                    sk = ("xsf", i2)
                else:
                    self.act(BC[:, ct - 12, :], acc[:, i2, :], AF.Silu, [("acc", i2), "params"], [("BC", ct - 12)], bias=p0[:, 40 + ct:41 + ct])
                    src = BC[:, ct - 12, :]
                    sk = ("BC", ct - 12)
                if ct < 16:
                    for b in range(NB):
                        self.tr(self.psT[:, b * 128:(b + 1) * 128], src[:, b * 128:(b + 1) * 128], self.ident_bf, [sk, "consts"], [("ps", 7)])
                    dst = xs_tok[:, :, ct * 128:(ct + 1) * 128] if ct < 12 else B_tok[:, :, (ct - 12) * 128:(ct - 11) * 128]
                    self.cp("act", dst, self.psT[:, 0:512].rearrange("p (b t) -> p b t", t=128), [("ps", 7)], [("xstok", ct)])
            self.cp("pool", self.chist[:], xpre[:, :, TT:TT + 3], [("xpre", ct) for ct in range(20)], ["chist"])
            self.S.barrier()
            esB.close()
            ymixB = E(self.sb("ymix0b", [128, 12, TT], BF16))
            sz = E(self.sb("sz_tok", [128, NB, 1536], BF16))
            dtt = E(self.sb("dt_tok", [128, NB, 24], F32))
            dtA = E(self.sb("dtA", [128, NB, 24], F32))
            for cb in range(6):
                wv, wk = self.stream_w(self.w_in0, 3072 + cb * 256, 256)
                for b in range(NB):
                    pb = self.gb % 4
                    self.gb += 1
                    for k in range(KT):
                        self.mm(ps[pb][:, 0:256], xn[:, k, b * 128:(b + 1) * 128], wv[:, k, :], k == 0, k == KT - 1, [wk, ("xn", k)], [("ps", pb)])
                    self.act(sz[:, b, cb * 256:(cb + 1) * 256], ps[pb][:, 0:256], AF.Silu, [("ps", pb)], [("sz", b)])
            wv, wk = self.stream_w(self.w_in0, 4608, 24)
            for b in range(NB):
                pb = self.gb % 4
                self.gb += 1
                for k in range(KT):
                    self.mm(ps[pb][:, 0:24], xn[:, k, b * 128:(b + 1) * 128], wv[:, k, :], k == 0, k == KT - 1, [wk, ("xn", k)], [("ps", pb)])
                self.tt("dve", dtt[:, b, :], ps[pb][:, 0:24], p0[:, 284:308], ALU.add, [("ps", pb), "params"], ["dtt"])
            self.act(dtt[:], dtt[:], AF.Exp, ["dtt"], ["dtt"])
            self.act(dtt[:], dtt[:], AF.Ln, ["dtt"], ["dtt"], bias=self.one_col)
            self.tt("dve", dtA[:], dtt[:], self.Arow[:].unsqueeze(1).broadcast_to([128, NB, 24]), ALU.mult, ["dtt", "Arow"], ["dtA"])
            cbs = E(self.sb("cb_sb", [128, 2, 128], BF16))
            rhsD = E(self.sb("rhsD", [128, 6, 128], F32))
            LE = E(self.sb("LE", [128, 6, 256], F32))
            MT = E(self.sb("MT", [128, 6, 128], BF16))
            CTs = E(self.sb("CTs", [128, 6, 128], BF16))
            xdt = E(self.sb("xdt", [128, 6, 2, 64], BF16))
            yg = E(self.sb("ssd_yg", [128, 2, 384], F32))
            ss = E(self.sb("ssd_ss", [128, 4], F32))
            yn = E(self.sb("ssd_yn", [128, 2, 384], BF16))
            hc = 0
            for b in range(NB):
                bs = slice(b * 128, (b + 1) * 128)
                for g in range(4):
                    gi = g % 2
                    self.mm(ps[4][:, 0:128], BC[:, g, bs], BC[:, 4 + g, bs], True, True, [("BC", g), ("BC", 4 + g)], [("ps", 4)])
                    self.cp("act", cbs[:, gi, :], ps[4][:, 0:128], [("ps", 4)], [("cbs", gi)])
                    DB = (0, 1, 5)
                    ybank = 2 + gi
                    hs = list(range(6))
                    for r in hs:
                        hh = g * 6 + r
                        self.ts("dve", rhsD[:, r, :], self.TriF, dtA[:, b, hh:hh + 1], None, ALU.mult, None, ["dtA", "consts"], [("rhsD", r)])
                    for r in hs:
                        db = DB[r // 2]
                        o = (r % 2) * 256
                        self.mm(ps[db][:, o:o + 128], self.UstrF, rhsD[:, r, :], True, False, [("rhsD", r), "consts"], [("ps", db)])
                        self.mm(ps[db][:, o:o + 128], self.ident_f, self.negmF, False, True, ["consts"], [("ps", db)])
                        self.mm(ps[db][:, o + 128:o + 256], self.ones_f, rhsD[:, r, :], True, True, [("rhsD", r), "consts"], [("ps", db)])
                    for r in hs:
                        o = (r % 2) * 256
                        self.act(LE[:, r, :], ps[DB[r // 2]][:, o:o + 256], AF.Exp, [("ps", DB[r // 2])], [("LE", r)])
                    for r in hs:
                        hh = g * 6 + r
                        xsl = xs_tok[:, b, hh * 64:(hh + 1) * 64]
                        self.tt("dve", MT[:, r, :], cbs[:, gi, :], LE[:, r, 0:128], ALU.mult, [("cbs", gi), ("LE", r)], [("MT", r)])
                        self.ts("dve", xdt[:, r, 0, :], xsl, dtt[:, b, hh:hh + 1], None, ALU.mult, None, [("xstok", hh // 2), "dtt"], [("xdt", r)])
                        self.ts("dve", xdt[:, r, 1, :], xdt[:, r, 0, :], LE[:, r, 127:128], None, ALU.mult, None, [("xdt", r), ("LE", r)], [("xdtw", r)])
                        self.tt("dve", CTs[:, r, :], BC[:, 4 + g, bs], LE[:, r, 128:256], ALU.mult, [("BC", 4 + g), ("LE", r)], [("CTs", r)])
                    for r in hs:
                        hh = g * 6 + r
                        yc = slice(r * 64, (r + 1) * 64)
                        self.mm(ps[ybank][:, yc], MT[:, r, :], xdt[:, r, 0, :], True, False, [("MT", r), ("xdt", r)], [("ps", ybank)])
                        self.mm(ps[ybank][:, yc], CTs[:, r, :], self.STb[:, hh, :], False, True, [("CTs", r), ("STb", hh)], [("ps", ybank)])
                        self.mm(ps[6][:, yc], B_tok[:, b, g * 128:(g + 1) * 128], xdt[:, r, 1, :], True, True, [("xstok", 12 + g), ("xdtw", r)], [("ps", 6)])
                    for r in hs:
                        hh = g * 6 + r
                        yc = slice(r * 64, (r + 1) * 64)
                        xsl = xs_tok[:, b, hh * 64:(hh + 1) * 64]
                        self.stt("dve", self.STf[:, hh, :], self.STf[:, hh, :], LE[:, r, 255:256], ps[6][:, yc], ALU.mult, ALU.add,
                                 [("STf", hh), ("LE", r), ("ps", 6)], [("STf", hh)])
                        self.cp("act", self.STb[:, hh, :], self.STf[:, hh, :], [("STf", hh)], [("STb", hh)])
                        self.stt("dve", yg[:, gi, yc], xsl, p0[:, 164 + hh:165 + hh], ps[ybank][:, yc], ALU.mult, ALU.add,
                                 [("xstok", hh // 2), "params", ("ps", ybank)], [("yg", gi)])
                    gsl = slice(g * 384, (g + 1) * 384)
                    self.tt("dve", yg[:, gi, :], yg[:, gi, :], sz[:, b, gsl], ALU.mult, [("yg", gi), ("sz", b)], [("yg", gi)])
                    self.act(yn[:, gi, :], yg[:, gi, :], AF.Square, [("yg", gi)], [("yn", gi)])
                    self.S.add("dve", lambda e_, g=g, gi=gi: e_.reduce_sum(out=ss[:, g:g + 1], in_=yn[:, gi, :], axis=mybir.AxisListType.X), [("yn", gi)], [("ss", g)])
                    self.act(ss[:, g:g + 1], ss[:, g:g + 1], AF.Ln, [("ss", g)], [("ss", g)], scale=1.0 / 384, bias=self.eps_col)
                    self.act(ss[:, g:g + 1], ss[:, g:g + 1], AF.Exp, [("ss", g)], [("ss", g)], scale=-0.5)
                    self.stt("dve", yn[:, gi, :], yg[:, gi, :], ss[:, g:g + 1], self.nw[:, gsl], ALU.mult, ALU.mult, [("yg", gi), ("ss", g), "params"], [("yn", gi)])
                    for i in range(3):
                        self.tr(self.psT[:, i * 128:(i + 1) * 128], yn[:, gi, i * 128:(i + 1) * 128], self.ident_bf, [("yn", gi), "consts"], [("ps", 7)])
                    self.cp("act", ymixB[:, 3 * g:3 * g + 3, bs], self.psT[:, 0:384].rearrange("p (k t) -> p k t", t=128),
                            [("ps", 7)], [("ymix", 4 + 3 * g + i) for i in range(3)])
            if self.dbg == "ymix0":
                self.dma("pool", self.dbg_d[:, 0:4, ti * TT:(ti + 1) * TT], ymix[:], [("ymix", i) for i in range(KT)], ["dbgout"])
                self.dma("pool", self.dbg_d[:, 4:16, ti * TT:(ti + 1) * TT], ymixB[:], [("ymix", i) for i in range(KT)], ["dbgout2"])
            self.add_resid(self.w_out0, [ymix[:, i, :] for i in range(4)] + [ymixB[:, i, :] for i in range(12)], KT, [("ymix", i) for i in range(KT)])
        self.S.barrier()
        with ExitStack() as es:
            self.ffn(p0[:, 16:32], self.w1_0, self.w3_0, self.w2_0, es)
        self.S.barrier()

    def build(self):
        nc = self.nc
        T = self.T
        xT = self.dram_in("xT", [D, T])
        outT = nc.dram_tensor("outT", [D, T], F32, kind="ExternalOutput").ap()
        cst = self.dram_in("consts", [128, 3208])
        self.wpack = self.dram_in("wpack", [128, WTOT])
        self.wscr = nc.dram_tensor("wscr", [128, WTOT], BF16, kind="Internal").ap()
        if self.dbg:
            self.dbg_d = nc.dram_tensor("dbg", [128, 16, T], F32, kind="ExternalOutput").ap()
        with ExitStack() as es:
            E = es.enter_context
            self.h = E(self.sb("h", [128, KT, TT], F32))
            self.xn = E(self.sb("xn", [128, KT, TT], BF16))
            self.sq = E(self.sb("sq", [128, 2, TT], BF16))
            self.rstd = E(self.sb("rstd", [128, TT], F32))
            self.wbuf = [E(self.sb("wbuf%d" % i, [128, 4096], BF16)) for i in range(2)]
            self.ps = [E(nc.psum_tensor("ps%d" % i, [128, 512], F32)) for i in range(7)]
            self.psT = E(nc.psum_tensor("psT", [128, 1024], BF16))
            self.gb = 0
            cbf = E(self.sb("cbf", [128, 1664], BF16))
            self.load(cbf[:], cst[:, 0:1664], ["consts"], cast=True)
            self.ident_bf = cbf[:, 0:128]
            self.ones_bf = cbf[:, 128:256]
            self.bones_bf = cbf[:, 256:384]
            self.onespad = cbf[:, 384:640].rearrange("p (e m) -> p e m", m=128)
            self.maskPC = cbf[:, 640:1664].rearrange("p (k m) -> p k m", m=512)
            cf = E(self.sb("cf", [128, 1544], F32))
            self.load(cf[:], cst[:, 1664:3208], ["consts"])
            self.gmask = cf[:, 0:128]
            self.segm = cf[:, 128:640]
            self.eps_col = cf[:, 640:641]
            self.one_col = cf[:, 641:642]
            self.cf = cf
            if 1 in self.layers:
                self.layer1_setup(es)
            if 0 in self.layers:
                self.layer0_setup(es)
            self.S.barrier(full=True)
            for ti in range(self.NT):
                t0 = ti * TT
                for k in range(KT):
                    self.dma("sp", self.h[:, k, :], xT[k * 128:(k + 1) * 128, t0:t0 + TT], (), [("h", k)])
                if 0 in self.layers:
                    self.layer0(ti)
                if 1 in self.layers:
                    self.layer1(ti)
                outs = []
                for k in range(KT):
                    outs.append(self.S.add("sp", (lambda e, k=k, t0=t0: e.dma_start(out=outT[k * 128:(k + 1) * 128, t0:t0 + TT], in_=self.h[:, k, :])),
                                           [("h", k)], [("out", ti, k)], dma=True))
                self.S.barrier(full=(ti == self.NT - 1))
            self.S.emit(nc, es)
        return nc


def make_consts():
    c = np.zeros((128, 3208), np.float32)
    p = np.arange(128)
    c[:, 0:128] = np.eye(128)
    c[:, 128:256] = 1.0
    c[:, 256:384] = (p[:, None] // 64 == p[None, :] // 64)
    for e in range(2):
        c[:, 384 + e * 128 + 64 * e:384 + e * 128 + 64 * e + 64] = 1.0
    k = p[:, None]
    q = p[None, :]
    mP = np.where(k > q, 0.0, -30000.0)
    mC = np.where(k <= q, 0.0, -30000.0)
    c[:, 640:1152] = np.tile(mP, (1, 4))
    c[:, 1152:1664] = np.tile(mC, (1, 4))
    o = 1664
    s = p[:, None]
    l = p[None, :]
    c[:, o:o + 128] = ((s // 64 == l // 64) & (l >= s))
    t = np.arange(512)
    c[:, o + 128:o + 640] = (t % 64 != 0)[None, :]
    c[:, o + 640] = EPS
    c[:, o + 641] = 1.0
    c[:, o + 642] = np.where(p < 64, 1.0, -1.0)
    for q_ in range(4):
        c[:, o + 643 + q_] = ((p // 16) % 4 == q_)
    c[:, o + 648:o + 776] = (s <= l)
    c[:, o + 776:o + 904] = (s > l)
    c[:, o + 904:o + 1032] = np.where(l < s, -30000.0, 0.0)
    c[:, o + 1032:o + 1160] = np.eye(128)
    c[:, o + 1160:o + 1288] = 1.0
    c[:, o + 1288:o + 1416] = (l == (s + 64) % 128)
    c[:, o + 1416:o + 1544] = np.arange(128)[None, :]
    return c


def prep_layer1(inp):
    f = lambda a: np.asarray(a, np.float32)
    w = f(inp["w_in1"])
    qc, kc, vc, qdc, kdc, vdc, goc, glc = np.split(w, np.cumsum([1024, 256, 256, 512, 512, 1024, 1024, 16])[:-1], axis=1)
    order = []
    for j in range(2):
        for r in range(4):
            for e in range(2):
                hh = (2 * j + e) * 4 + r
                order.extend(range(hh * 64, hh * 64 + 64))
    order = np.array(order)
    w_in1p = np.concatenate([qc[:, order], kc, qdc, kdc, goc, glc, vc, vdc], axis=1)
    wo = f(inp["w_out1"])
    w_out1p = np.concatenate([wo[0:1024][order], wo[1024:]], axis=0)
    p1 = np.zeros((128, 64), np.float32)
    p1[:, 0:16] = f(inp["norm1_mix"]).reshape(16, 128).T
    p1[:, 16:32] = f(inp["norm1_ffn"]).reshape(16, 128).T
    p1[:, 32] = np.tile(f(inp["swa_q_norm"]), 2)
    p1[:, 33] = np.tile(f(inp["swa_k_norm"]), 2)
    sinks = f(inp["swa_sinks"])
    for j in range(2):
        for r in range(4):
            for e in range(2):
                p1[64 * e:64 * e + 64, 34 + j * 4 + r] = sinks[(2 * j + e) * 4 + r]
    p1[:, 42:46] = f(inp["gla_gate_b"]).reshape(4, 128).T
    p1[:, 46:48] = f(inp["gla_norm_w"]).reshape(2, 128).T
    return {
        "w_in1p": np.ascontiguousarray(w_in1p), "w_out1p": np.ascontiguousarray(w_out1p),
        "ffn1_w1": f(inp["ffn1_w1"]), "ffn1_w3": f(inp["ffn1_w3"]), "ffn1_w2": f(inp["ffn1_w2"]),
        "params1": p1, "gla_gate_w2": f(inp["gla_gate_w2"]),
    }


def prep_layer0(inp):
    f = lambda a: np.asarray(a, np.float32)
    w = f(inp["w_in0"])
    u, z, xbc, dtc = np.split(w, np.cumsum([512, 1536, 2560, 24])[:-1], axis=1)
    w_in0p = np.concatenate([u, xbc, z, dtc], axis=1)
    p = np.arange(128)
    p0 = np.zeros((128, 320), np.float32)
    p0[:, 0:16] = f(inp["norm0_mix"]).reshape(16, 128).T
    p0[:, 16:32] = f(inp["norm0_ffn"]).reshape(16, 128).T
    p0[:, 32:36] = f(inp["s5_D"]).reshape(4, 128).T
    p0[:, 36:40] = f(inp["s5_glu_b"]).reshape(4, 128).T
    p0[:, 40:60] = f(inp["ssd_conv_b"]).reshape(20, 128).T
    cw = f(inp["ssd_conv_w"])
    p0[:, 60:140] = cw.reshape(4, 20, 128).transpose(2, 1, 0).reshape(128, 80)
    p0[:, 140:164] = f(inp["ssd_A_log"])[None, :]
    p0[:, 164:188] = f(inp["ssd_D"])[None, :]
    p0[:, 188:220] = f(inp["s5_A_re"])[:, p % 64].T
    p0[:, 220:252] = f(inp["s5_A_im"])[:, p % 64].T
    p0[:, 252:284] = f(inp["s5_log_dt"])[None, :]
    p0[:, 284:308] = f(inp["ssd_dt_bias"])[None, :]
    gidx = (8 * np.arange(4)[None, :] + (p // 16)[:, None])
    ch = np.zeros((128, 5, 4, 64), np.float32)
    ch[:, 0] = f(inp["s5_A_re"])[gidx]
    ch[:, 1] = f(inp["s5_A_im"])[gidx]
    ch[:, 2] = f(inp["s5_log_dt"])[gidx][:, :, None]
    ch[:, 3] = f(inp["s5_B_re"])[gidx, :, (p % 16)[:, None]]
    ch[:, 4] = f(inp["s5_B_im"])[gidx, :, (p % 16)[:, None]]
    Cre, Cim = f(inp["s5_C_re"]), f(inp["s5_C_im"])
    s5C = np.zeros((128, 32, 2, 16), np.float32)
    s5C[0:64, :, 0, :] = Cre.transpose(2, 0, 1)
    s5C[64:128, :, 0, :] = Cim.transpose(2, 0, 1)
    s5C[0:64, :, 1, :] = Cim.transpose(2, 0, 1)
    s5C[64:128, :, 1, :] = Cre.transpose(2, 0, 1)
    return {
        "w_in0p": np.ascontiguousarray(w_in0p), "w_out0": f(inp["w_out0"]),
        "ffn0_w1": f(inp["ffn0_w1"]), "ffn0_w3": f(inp["ffn0_w3"]), "ffn0_w2": f(inp["ffn0_w2"]),
        "params0": p0, "s5ch": np.ascontiguousarray(ch.reshape(128, 5, 256)), "s5C": s5C,
        "ssd_nw": np.ascontiguousarray(np.broadcast_to(f(inp["ssd_norm_w"])[None, :], (128, 1536))),
        "s5_glu_w": f(inp["s5_glu_w"]),
    }


def pack_weights(arrs, wreg):
    wp = np.zeros((128, WTOT), np.float32)
    for pieces, k0, nk, off, n in wreg:
        blk = np.concatenate([arrs[nm][k0 * 128:(k0 + nk) * 128, c0:c0 + nc_] for (nm, c0, nc_) in pieces], axis=1)
        wp[:, off:off + n] = blk.reshape(nk, 128, -1).transpose(1, 0, 2).reshape(128, n)
    return wp


_CACHE = {}


def kernel(**inputs):
    x = np.asarray(inputs["x"], np.float32)
    Bn, T, _ = x.shape
    if "nc" not in _CACHE:
        b = Builder(T, (0, 1))
        _CACHE["nc"] = b.build()
        _CACHE["names"] = set(b.din.keys())
        _CACHE["wreg"] = b.wreg
    nc = _CACHE["nc"]
    common = {"consts": make_consts()}
    common.update(prep_layer0(inputs))
    common.update(prep_layer1(inputs))
    common["wpack"] = pack_weights(common, _CACHE["wreg"])
    common = {k: v for k, v in common.items() if k in _CACHE["names"]}
    in_maps = []
    for i in range(Bn):
        m = dict(common)
        m["xT"] = np.ascontiguousarray(x[i].T)
        in_maps.append(m)
    res = run_bass_kernel_spmd(nc, in_maps, core_ids=list(range(Bn)))
    out = np.empty((Bn, T, D), np.float32)
    for i in range(Bn):
        out[i] = np.asarray(res.results[i]["outT"]).T
    return out
```
